# Optimizing a Trainium2 kernel written in Bass

```python
import math
import jax, jax.numpy as jnp
from jax import lax
import numpy as np

D_MODEL = 2048
BATCH = 4
SEQ = 4096
DEPTH = 1

D_MIX = D_MODEL
GLA_WIDTH = D_MIX // 2
GLA_HEADS = 4
GLA_KEY_WIDTH = GLA_WIDTH // 2
GLA_HEAD_K = GLA_KEY_WIDTH // GLA_HEADS
GLA_HEAD_V = GLA_WIDTH // GLA_HEADS
GLA_GATE_RANK = 16
GLA_GATE_TAU = 16.0
GLA_CHUNK = 64
SSD_WIDTH = D_MIX - GLA_WIDTH
SSD_HEAD_DIM = 64
SSD_HEADS = SSD_WIDTH // SSD_HEAD_DIM
SSD_GROUPS = 4
SSD_HEADS_PER_GROUP = SSD_HEADS // SSD_GROUPS
SSD_STATE = 128
SSD_CONV = 4
SSD_CHUNK = 128
XBC_DIM = SSD_WIDTH + 2 * SSD_GROUPS * SSD_STATE
IN_SIZES = (GLA_KEY_WIDTH, GLA_KEY_WIDTH, GLA_WIDTH, GLA_WIDTH, GLA_GATE_RANK,
            SSD_WIDTH, XBC_DIM, SSD_HEADS)
IN_DIM = sum(IN_SIZES)
IN_SPLITS = tuple(int(v) for v in np.cumsum(IN_SIZES)[:-1])
MOE_GROUPS = 4
EXPERTS_PER_GROUP = 8
N_EXPERTS = MOE_GROUPS * EXPERTS_PER_GROUP
TOP_K = 2
D_EXPERT = D_MODEL // 4
EPS = 1e-6

kernel_name = "hymba_gla_ssd_hiermoe"


def rmsnorm(x, w):
    xf = x.astype(jnp.float32)
    y = xf * lax.rsqrt(jnp.mean(xf * xf, axis=-1, keepdims=True) + EPS)
    return (y * w.astype(jnp.float32)).astype(x.dtype)


def gla_chunked(q, k, v, log_a):
    b, s, h, dk = q.shape
    dv = v.shape[-1]
    n = s // GLA_CHUNK
    q = q * (dk ** -0.5)

    def to_chunks(t):
        return t.reshape(b, n, GLA_CHUNK, h, t.shape[-1]).transpose(1, 0, 3, 2, 4)

    qc, kc, vc = to_chunks(q), to_chunks(k), to_chunks(v)
    gc = to_chunks(log_a.astype(jnp.float32))
    causal = jnp.tril(jnp.ones((GLA_CHUNK, GLA_CHUNK), bool))[:, :, None]

    def step(state, inp):
        qi, ki, vi, gi = inp
        G = jnp.cumsum(gi, axis=2)
        diff = G[:, :, :, None, :] - G[:, :, None, :, :]
        decay = jnp.exp(jnp.where(causal, diff, -jnp.inf))
        scores = jnp.einsum('bhid,bhjd,bhijd->bhij', qi, ki, decay)
        o_intra = jnp.einsum('bhij,bhjv->bhiv', scores, vi)
        o_inter = jnp.einsum('bhid,bhdv->bhiv', qi * jnp.exp(G), state)
        G_last = G[:, :, -1:, :]
        k_dec = ki * jnp.exp(G_last - G)
        state = state * jnp.exp(G_last[:, :, 0, :])[..., None] + jnp.einsum('bhjd,bhjv->bhdv', k_dec, vi)
        return state, o_intra + o_inter

    state0 = jnp.zeros((b, h, dk, dv), jnp.float32)
    _, o = lax.scan(step, state0, (qc, kc, vc, gc))
    return o.transpose(1, 0, 3, 2, 4).reshape(b, s, h, dv)


def segsum(a):
    T = a.shape[-1]
    rep = jnp.broadcast_to(a[..., :, None], a.shape + (T,))
    strict = jnp.tril(jnp.ones((T, T), bool), -1)
    cs = jnp.cumsum(jnp.where(strict, rep, 0.0), axis=-2)
    return jnp.where(jnp.tril(jnp.ones((T, T), bool)), cs, -jnp.inf)


def ssd_chunked(x, dt, a, bm, cm):
    b, s, g, r, p = x.shape
    nst = bm.shape[-1]
    c = s // SSD_CHUNK
    l = SSD_CHUNK
    xs = (x * dt[..., None]).reshape(b, c, l, g, r, p)
    dA = (dt * a).astype(jnp.float32).reshape(b, c, l, g, r).transpose(0, 3, 4, 1, 2)
    bm = bm.reshape(b, c, l, g, nst)
    cm = cm.reshape(b, c, l, g, nst)
    A_cs = jnp.cumsum(dA, axis=-1)
    L = jnp.exp(segsum(dA))
    cb = jnp.einsum('bclgn,bcsgn->bgcls', cm, bm)
    y_diag = jnp.einsum('bgrcls,bcsgrp->bclgrp', cb[:, :, None] * L, xs)
    decay_states = jnp.exp(A_cs[..., -1:] - A_cs).transpose(0, 3, 4, 1, 2)
    states = jnp.einsum('bclgn,bclgrp->bcgrpn', bm, xs * decay_states[..., None])
    states = jnp.concatenate([jnp.zeros_like(states[:, :1]), states], axis=1)
    chunk_decay = jnp.exp(segsum(jnp.pad(A_cs[..., -1], ((0, 0), (0, 0), (0, 0), (1, 0)))))
    new_states = jnp.einsum('bgrzc,bcgrpn->bzgrpn', chunk_decay, states)[:, :-1]
    out_decay = jnp.exp(A_cs).transpose(0, 3, 4, 1, 2)
    y_off = jnp.einsum('bclgn,bcgrpn->bclgrp', cm, new_states) * out_decay[..., None]
    return (y_diag + y_off).reshape(b, s, g, r, p)


def causal_depthwise_conv(u, w, bias):
    out = lax.conv_general_dilated(
        u, w[:, None, :].astype(u.dtype), window_strides=(1,),
        padding=[(SSD_CONV - 1, 0)], dimension_numbers=('NWC', 'WIO', 'NWC'),
        feature_group_count=u.shape[-1])
    return out + bias.astype(u.dtype)


def mixer_block(h, w_in, w_gk_up, b_gk, w_gla_norm, w_conv, b_conv, dt_bias, a_log,
                d_skip, w_ssd_norm, w_out):
    b, s, _ = h.shape
    proj = h @ w_in
    q, k, v, g_out, gk_lr, z, xbc, dt_raw = jnp.split(proj, IN_SPLITS, axis=-1)
    log_a = jax.nn.log_sigmoid((gk_lr @ w_gk_up + b_gk).astype(jnp.float32)) / GLA_GATE_TAU
    o = gla_chunked(q.reshape(b, s, GLA_HEADS, GLA_HEAD_K),
                    k.reshape(b, s, GLA_HEADS, GLA_HEAD_K),
                    v.reshape(b, s, GLA_HEADS, GLA_HEAD_V),
                    log_a.reshape(b, s, GLA_HEADS, GLA_HEAD_K)).astype(h.dtype)
    o = rmsnorm(o, w_gla_norm) * jax.nn.silu(g_out).reshape(b, s, GLA_HEADS, GLA_HEAD_V)
    o = o.reshape(b, s, GLA_WIDTH)
    xbc = jax.nn.silu(causal_depthwise_conv(xbc, w_conv, b_conv))
    xs, bm, cm = jnp.split(xbc, (SSD_WIDTH, SSD_WIDTH + SSD_GROUPS * SSD_STATE), axis=-1)
    dt = jax.nn.softplus((dt_raw + dt_bias).astype(jnp.float32))
    a = -jnp.exp(a_log.astype(jnp.float32))
    shp = (b, s, SSD_GROUPS, SSD_HEADS_PER_GROUP)
    xh = xs.reshape(shp + (SSD_HEAD_DIM,))
    y = ssd_chunked(xh, dt.reshape(shp), a.reshape(SSD_GROUPS, SSD_HEADS_PER_GROUP),
                    bm.reshape(b, s, SSD_GROUPS, SSD_STATE), cm.reshape(b, s, SSD_GROUPS, SSD_STATE))
    y = y + d_skip.reshape(SSD_GROUPS, SSD_HEADS_PER_GROUP)[..., None] * xh
    y = y.astype(h.dtype).reshape(b, s, SSD_WIDTH) * jax.nn.silu(z)
    y = rmsnorm(y.reshape(b, s, SSD_GROUPS, SSD_WIDTH // SSD_GROUPS),
                w_ssd_norm.reshape(SSD_GROUPS, SSD_WIDTH // SSD_GROUPS)).reshape(b, s, SSD_WIDTH)
    return jnp.concatenate([o, y], axis=-1) @ w_out


def hier_moe(h, w_rg, b_rg, w_re, b_re, w_gate, w_up, w_down):
    shape = h.shape
    t = h.reshape(-1, shape[-1])
    T = t.shape[0]
    g_prob = jax.nn.softmax((t @ w_rg + b_rg).astype(jnp.float32), axis=-1)
    g_w, g_idx = lax.top_k(g_prob, 1)
    e_logits = (t @ w_re + b_re).astype(jnp.float32).reshape(T, MOE_GROUPS, EXPERTS_PER_GROUP)
    e_sel = e_logits[jnp.arange(T), g_idx[:, 0]]
    e_prob = jax.nn.softmax(e_sel, axis=-1)
    e_w, e_idx = lax.top_k(e_prob, TOP_K)
    weights = g_w * e_w / jnp.sum(e_w, axis=-1, keepdims=True)
    expert = g_idx * EXPERTS_PER_GROUP + e_idx
    flat_e = expert.reshape(-1)
    flat_w = weights.reshape(-1)
    flat_tok = jnp.repeat(jnp.arange(T), TOP_K)
    order = jnp.argsort(flat_e)
    sorted_tok = flat_tok[order]
    group_sizes = jnp.bincount(flat_e, length=N_EXPERTS).astype(jnp.int32)
    xs = t[sorted_tok]
    hg = lax.ragged_dot(xs, w_gate, group_sizes)
    hu = lax.ragged_dot(xs, w_up, group_sizes)
    y = lax.ragged_dot(jax.nn.silu(hg) * hu, w_down, group_sizes)
    y = y * flat_w[order][:, None].astype(y.dtype)
    out = jnp.zeros((T, shape[-1]), y.dtype).at[sorted_tok].add(y)
    return out.astype(h.dtype).reshape(shape)


def setup_inputs(seed: int = 0) -> dict:
    key = jax.random.key(seed)
    ks = jax.random.split(key, 24)
    f32 = jnp.float32
    D = D_MODEL

    def nrm(k, shape, fan_in):
        return jax.random.normal(k, shape, f32) * (fan_in ** -0.5)

    def gain(k, shape):
        return 1.0 + 0.02 * jax.random.normal(k, shape, f32)

    dt0 = jnp.exp(jax.random.uniform(ks[8], (DEPTH, SSD_HEADS), f32, math.log(1e-3), math.log(1e-1)))
    return {
        "x": jax.random.normal(ks[0], (BATCH, SEQ, D), f32),
        "w_norm_mix": gain(ks[1], (DEPTH, D)),
        "w_in": nrm(ks[2], (DEPTH, D, IN_DIM), D),
        "w_gk_up": nrm(ks[3], (DEPTH, GLA_GATE_RANK, GLA_KEY_WIDTH), GLA_GATE_RANK),
        "b_gk": 0.1 * jax.random.normal(ks[4], (DEPTH, GLA_KEY_WIDTH), f32),
        "w_gla_norm": gain(ks[5], (DEPTH, GLA_HEAD_V)),
        "w_conv": nrm(ks[6], (DEPTH, SSD_CONV, XBC_DIM), SSD_CONV),
        "b_conv": 0.02 * jax.random.normal(ks[7], (DEPTH, XBC_DIM), f32),
        "dt_bias": dt0 + jnp.log(-jnp.expm1(-dt0)),
        "a_log": jnp.log(jax.random.uniform(ks[9], (DEPTH, SSD_HEADS), f32, 1.0, 16.0)),
        "d_skip": 1.0 + 0.02 * jax.random.normal(ks[10], (DEPTH, SSD_HEADS), f32),
        "w_ssd_norm": gain(ks[11], (DEPTH, SSD_WIDTH)),
        "w_out": nrm(ks[12], (DEPTH, D_MIX, D), D_MIX),
        "w_norm_ffn": gain(ks[13], (DEPTH, D)),
        "w_router_group": nrm(ks[14], (DEPTH, D, MOE_GROUPS), D),
        "b_router_group": 0.01 * jax.random.normal(ks[15], (DEPTH, MOE_GROUPS), f32),
        "w_router_expert": nrm(ks[16], (DEPTH, D, N_EXPERTS), D),
        "b_router_expert": 0.01 * jax.random.normal(ks[17], (DEPTH, N_EXPERTS), f32),
        "w_gate": nrm(ks[18], (DEPTH, N_EXPERTS, D, D_EXPERT), D),
        "w_up": nrm(ks[19], (DEPTH, N_EXPERTS, D, D_EXPERT), D),
        "w_down": nrm(ks[20], (DEPTH, N_EXPERTS, D_EXPERT, D), D_EXPERT),
        "w_final_norm": gain(ks[21], (D,)),
    }


def reference(x, w_norm_mix, w_in, w_gk_up, b_gk, w_gla_norm, w_conv, b_conv, dt_bias, a_log,
              d_skip, w_ssd_norm, w_out, w_norm_ffn, w_router_group, b_router_group,
              w_router_expert, b_router_expert, w_gate, w_up, w_down, w_final_norm):
    for layer in range(DEPTH):
        h = rmsnorm(x, w_norm_mix[layer])
        x = x + mixer_block(h, w_in[layer], w_gk_up[layer], b_gk[layer], w_gla_norm[layer],
                            w_conv[layer], b_conv[layer], dt_bias[layer], a_log[layer],
                            d_skip[layer], w_ssd_norm[layer], w_out[layer])
        h = rmsnorm(x, w_norm_ffn[layer])
        x = x + hier_moe(h, w_router_group[layer], b_router_group[layer], w_router_expert[layer],
                         b_router_expert[layer], w_gate[layer], w_up[layer], w_down[layer])
    return rmsnorm(x, w_final_norm)
```

```python
import numpy as np
import ml_dtypes
from contextlib import ExitStack
import concourse.bass as bass
import concourse.mybir as mybir
from concourse.bass_utils import run_bass_kernel_spmd

F32 = mybir.dt.float32
BF16 = mybir.dt.bfloat16
I32 = mybir.dt.int32
AF = mybir.ActivationFunctionType
ALU = mybir.AluOpType
AX = mybir.AxisListType

ENGS = ("pe", "act", "dve", "pool", "sp")
ENG_ATTR = {"pe": "tensor", "act": "scalar", "dve": "vector", "pool": "gpsimd", "sp": "sync"}

D = 2048
SEQ = 4096
NT_FULL = 32
NT_HALF = 16
CAP = 256
NEXP = 32
EPS = 1e-6
NSLOT = NEXP * CAP


class Res:
    __slots__ = ("name", "w", "r")

    def __init__(self, name):
        self.name = name
        self.w = {}
        self.r = {}


class Chan:
    def __init__(self, key):
        self.key = key
        self.n = 0


class Buf:
    def __init__(self, t, name):
        self.t = t
        self.r = Res(name)

    def __getitem__(self, k):
        return self.t[k]


class Prog:
    def __init__(self, nc):
        self.nc = nc
        self.ops = {e: [] for e in ENGS}
        self.cnt = {e: 0 for e in ENGS}
        self.waited = {e: {} for e in ENGS}
        self.chans = []
        self.stack = ExitStack()
        self.alloc = self.stack
        self.uid = 0
        self.banks = []
        self.tps = []
        self.bi = 0
        self.ti = 0
        self.ncc = 0

    def sb(self, name, shape, dtype):
        self.uid += 1
        nm = "sb%d_%s" % (self.uid, name)
        return Buf(self.alloc.enter_context(self.nc.sbuf_tensor(nm, list(shape), dtype)), nm)

    def ps(self, name, shape, dtype):
        return Buf(self.stack.enter_context(self.nc.psum_tensor("ps_" + name, list(shape), dtype)), name)

    def chan(self):
        c = Chan(len(self.chans))
        self.chans.append(c)
        return c

    def bank(self):
        b = self.banks[self.bi % len(self.banks)]
        self.bi += 1
        return b

    def tpbank(self):
        b = self.tps[self.ti % len(self.tps)]
        self.ti += 1
        return b

    def _deps(self, reads, writes, accw):
        d = {}

        def add(k, v):
            if d.get(k, 0) < v:
                d[k] = v
        for r in list(reads) + list(accw):
            for k, v in r.w.items():
                add(k, v)
        for w in writes:
            for k, v in w.w.items():
                add(k, v)
            for k, v in w.r.items():
                add(k, v)
        return d

    def _commit(self, tok, reads, writes, accw):
        k, v = tok
        for r in reads:
            if r.r.get(k, 0) < v:
                r.r[k] = v
        for w in writes:
            w.w = {k: v}
            w.r = {}
        for w in accw:
            if w.w.get(k, 0) < v:
                w.w[k] = v

    def _waitlist(self, eng, d, skip_self):
        wl = []
        for k, v in d.items():
            if skip_self and k == eng:
                continue
            if self.waited[eng].get(k, 0) >= v:
                continue
            self.waited[eng][k] = v
            wl.append((k, v))
        return wl

    def op(self, eng, fns, reads=(), writes=()):
        if callable(fns):
            fns = [fns]
        d = self._deps(reads, writes, ())
        wl = self._waitlist(eng, d, skip_self=(eng == "pe"))
        self.cnt[eng] += 1
        self._commit((eng, self.cnt[eng]), reads, writes, ())
        self.ops[eng].append((wl, fns, None))

    def dma(self, eng, fn, chan, reads=(), writes=(), accw=()):
        d = self._deps(reads, writes, accw)
        wl = self._waitlist(eng, d, skip_self=False)
        chan.n += 1
        self._commit((("c", chan.key), chan.n * 16), reads, writes, accw)
        self.ops[eng].append((wl, [fn], chan))

    def collective(self, fn, reads=(), writes=()):
        d = self._deps(reads, writes, ())
        wl = self._waitlist("pool", d, skip_self=False)
        key = ("cc", self.ncc)
        self.ncc += 1
        self._commit((key, 1), reads, writes, ())
        self.ops["pool"].append((wl, [fn], key))

    def barrier(self):
        for e in ENGS:
            d = {}
            for e2 in ENGS:
                if e2 != "sp" and self.cnt[e2]:
                    d[e2] = self.cnt[e2]
            for c in self.chans:
                if c.n:
                    d[("c", c.key)] = c.n * 16
            for i in range(self.ncc):
                d[("cc", i)] = 1
            wl = self._waitlist(e, d, skip_self=False)
            self.ops[e].append((wl, [], None))

    def emit(self):
        nc = self.nc
        with ExitStack() as st:
            sems = {}
            for e in ENGS:
                sems[e] = st.enter_context(nc.semaphore("s_" + e))
            for i in range(self.ncc):
                sems[("cc", i)] = st.enter_context(nc.semaphore("s_cc%d" % i))
            for c in self.chans:
                sems[("c", c.key)] = st.enter_context(nc.semaphore("c%d" % c.key))
            block = st.enter_context(nc.Block())
            for e in ENGS:
                def body(eng, e=e):
                    for wl, fns, chan in self.ops[e]:
                        for k, v in wl:
                            eng.wait_ge(sems[k], v)
                        if not fns:
                            continue
                        ins = None
                        for f in fns:
                            ins = f(eng)
                        if chan is None:
                            ins.then_inc(sems[e], 1)
                        elif isinstance(chan, tuple):
                            ins.then_inc(sems[chan])
                        else:
                            ins.then_inc(sems[("c", chan.key)], 16)
                    if e == "sp":
                        for c in self.chans:
                            if c.n:
                                eng.wait_ge(sems[("c", c.key)], c.n * 16)
                        for e2 in ENGS:
                            if e2 != "sp" and self.cnt[e2]:
                                eng.wait_ge(sems[e2], self.cnt[e2])
                getattr(block, ENG_ATTR[e])(body)


def MM(out, lhsT, rhs, start=True, stop=True):
    return lambda e: e.matmul(out, lhsT=lhsT, rhs=rhs, start=start, stop=stop)


def TR(out, in_, ident):
    return lambda e: e.transpose(out=out, in_=in_, identity=ident)


def ACT(out, in_, func, **kw):
    return lambda e: e.activation(out=out, in_=in_, func=func, **kw)


def ACP(out, in_):
    return lambda e: e.copy(out=out, in_=in_)


def AMUL(out, in_, m):
    return lambda e: e.mul(out=out, in_=in_, mul=m)


def TT(out, a, b, op):
    return lambda e: e.tensor_tensor(out=out, in0=a, in1=b, op=op)


def TS(out, a, s1, s2, op0, op1=None):
    if op1 is None:
        return lambda e: e.tensor_scalar(out=out, in0=a, scalar1=s1, scalar2=None, op0=op0)
    return lambda e: e.tensor_scalar(out=out, in0=a, scalar1=s1, scalar2=s2, op0=op0, op1=op1)


def STT(out, a, s, b, op0, op1):
    return lambda e: e.scalar_tensor_tensor(out=out, in0=a, scalar=s, in1=b, op0=op0, op1=op1)


def CP(out, in_):
    return lambda e: e.tensor_copy(out=out, in_=in_)


def MSET(ap, v):
    return lambda e: e.memset(ap, v)


def RECIP(out, in_):
    return lambda e: e.reciprocal(out=out, in_=in_)


def RMAX(out, in_):
    return lambda e: e.reduce_max(out=out, in_=in_, axis=AX.X)


def RSUM(out, in_):
    return lambda e: e.reduce_sum(out=out, in_=in_, axis=AX.X)


def DMA(out, in_):
    return lambda e: e.dma_start(out=out, in_=in_)


_BREG = {}


def _bound_reg(e, bound):
    key = (id(e), bound)
    if key not in _BREG:
        reg = e.alloc_register("bnd%d" % len(_BREG))
        e.reg_mov(reg, bound)
        _BREG[key] = reg
    return _BREG[key]


def GATHER(out, src, idx, bound):
    return lambda e: e.indirect_dma_start(out=out, out_offset=None, in_=src,
                                          in_offset=bass.IndirectOffsetOnAxis(ap=idx, axis=0),
                                          bounds_check=_bound_reg(e, bound), oob_is_err=False)


def SCATTER(dst, idx, src, bound):
    return lambda e: e.indirect_dma_start(out=dst, out_offset=bass.IndirectOffsetOnAxis(ap=idx, axis=0),
                                          in_=src, in_offset=None, bounds_check=_bound_reg(e, bound), oob_is_err=False)


def bc3(ap, m):
    n = ap.shape[1]
    return ap.unsqueeze(2).broadcast_to([128, n, m])


def v3(ap, d):
    return ap.rearrange("p (h d) -> p h d", d=d)


def build_program(dbg=False, stop_after=99):
    _BREG.clear()
    nc = bass.Bass("TRN2", target_bir_lowering=False)
    P = Prog(nc)

    def din(name, shape, dt=F32):
        return nc.dram_tensor(name, list(shape), dt, kind="ExternalInput")

    x_full = din("x_full", [SEQ, D])
    x_half = din("x_half", [2048, D])
    wn3_d = din("wn3", [3, 128, D])
    wg_in_d = din("wg_in", [D, 1808])
    ws_in_d = din("ws_in", [D, 1544])
    wout_d = din("wout", [D, D])
    wr_d = din("wr", [D, 36])
    cf_d = din("cf", [128, 1152])
    cb_d = din("cb", [128, 384], BF16)
    sel_d = din("sel8", [8, 1024])
    wgk_d = din("wgk", [33, 256])
    sm_d = din("smalls", [128, 1024])
    ridx_d = din("ridx", [128, 32], I32)
    ne_decl = NEXP if 3 <= stop_after <= 20 or stop_after == 99 else 1
    wgate_d = din("w_gate", [ne_decl, D, 512])
    wup_d = din("w_up", [ne_decl, D, 512])
    wdown_d = din("w_down", [ne_decl, 512, D])
    out_d = nc.dram_tensor("out", [2048, D], F32, kind="ExternalOutput")

    mixbuf = nc.dram_tensor("mixbuf", [SEQ, 1024], BF16)
    gath = nc.dram_tensor("gath", [2 * SEQ, 1024], BF16)
    x1buf = nc.dram_tensor("x1buf", [2048, D], F32)
    hs = nc.dram_tensor("hs", [NSLOT, D], BF16)
    ys = nc.dram_tensor("ys", [NSLOT, D], F32)
    R_mix = [Res("mixbuf%d" % i) for i in range(8)]; R_gath = [Res("gath%d" % i) for i in range(8)]; R_x1 = Res("x1buf"); R_hs = Res("hs"); R_ys = Res("ys")
    R_out = Res("out")
    if dbg:
        dbg_mix = nc.dram_tensor("dbg_mix", [SEQ, 1024], BF16, kind="ExternalOutput")
        dbg_x1 = nc.dram_tensor("dbg_x1", [2048, D], F32, kind="ExternalOutput")
        dbg_slots = nc.dram_tensor("dbg_slots", [128, 32], I32, kind="ExternalOutput")
        dbg_cw = nc.dram_tensor("dbg_cw", [128, 32], F32, kind="ExternalOutput")

    with P.stack:
        P.tps = [P.ps("tp%d" % i, [128, 1024], BF16) for i in range(2)]
        P.banks = [P.ps("mm%d" % i, [128, 512], F32) for i in range(6)]

        cf = P.sb("cf", [128, 1152], F32)
        cbf = P.sb("cbf", [128, 384], BF16)
        wn = P.sb("wn", [128, D], F32)
        xt = [P.sb("xt%d" % i, [128, D], F32) for i in range(2)]
        hb = [P.sb("hb%d" % i, [128, D], BF16) for i in range(2)]
        junk = P.sb("junk", [128, D], BF16)
        ss = [P.sb("ss%d" % i, [128, 4], F32) for i in range(2)]
        smalls = P.sb("smalls", [128, 1024], F32)
        slots_i = P.sb("slots_i", [128, 32], I32)
        cw = P.sb("cw", [128, 32], F32)
        ch_x = [P.chan(), P.chan()]
        ch_c = P.chan()
        ch_wn = P.chan()
        P.dma("sp", DMA(cf[:], cf_d.ap()), P.chan(), writes=[cf.r])
        P.dma("sp", DMA(cbf[:], cb_d.ap()), P.chan(), writes=[cbf.r])
        P.dma("sp", DMA(smalls[:], sm_d.ap()), P.chan(), writes=[smalls.r])
        ident = cbf[:, 0:128]
        TriLt_b = cbf[:, 128:256]
        ones_b = cbf[:, 256:384]
        TriInc = cf[:, 0:128]; TriGt = cf[:, 128:256]; NegMask = cf[:, 256:384]; IdentF = cf[:, 384:512]
        TriIncBD = cf[:, 512:640]; TriGtBD = cf[:, 640:768]; MaskBD2 = cf[:, 768:1024]
        eC = cf[:, 1024:1056]
        wgn = smalls[:, 0:256]
        wsn = smalls[:, 256:768]
        wc = smalls[:, 768:800]
        bcv = smalls[:, 800:808]
        dtb = smalls[:, 808:816]; alog = smalls[:, 816:824]; dsk = smalls[:, 824:832]
        brb = smalls[:, 832:868]
        SC_D = float(1.0 / np.sqrt(D))

        def load_wn(i):
            P.dma("sp", DMA(wn[:], wn3_d[i, :, :]), ch_wn, writes=[wn.r])

        def rmsnorm_to_bf16(src, slot, dst):
            s = ss[slot]
            P.op("dve", MSET(s[:, 0:1], 0.0), writes=[s.r])
            P.op("act", ACT(junk[:], src[:], AF.Square, scale=SC_D, accum_out=s[:, 0:1]),
                 reads=[src.r], writes=[junk.r, s.r])
            P.op("act", ACT(s[:, 1:2], s[:, 0:1], AF.Sqrt, bias=EPS), reads=[s.r], writes=[s.r])
            P.op("dve", RECIP(s[:, 2:3], s[:, 1:2]), reads=[s.r], writes=[s.r])
            P.op("dve", STT(dst[:], src[:], s[:, 2:3], wn[:], ALU.mult, ALU.mult),
                 reads=[src.r, s.r, wn.r], writes=[dst.r])

        def transpose16(src, dstT, col0):
            for half in range(2):
                tp = P.tpbank()
                P.op("pe", [TR(tp[:, c * 128:(c + 1) * 128], src[:, (half * 8 + c) * 128:(half * 8 + c + 1) * 128], ident)
                            for c in range(8)], reads=[src.r, cbf.r], writes=[tp.r])
                eng = "act" if half == 0 else "dve"
                f = ACP if eng == "act" else CP
                P.op(eng, f(dstT[:, half * 8:(half + 1) * 8, col0:col0 + 128], v3(tp[:, :], 128)),
                     reads=[tp.r], writes=[dstT.r])

        def front_block(blk, hT):
            for tt in range(4):
                t = blk * 4 + tt
                s = t % 2
                P.dma("sp", DMA(xt[s][:], x_full[t * 128:(t + 1) * 128, :]), ch_x[s], writes=[xt[s].r])
                rmsnorm_to_bf16(xt[s], s, hb[s])
                transpose16(hb[s], hT, tt * 128)

        def load_w(dram, ncols, name):
            W = P.sb(name, [128, 16, ncols], BF16)
            parts = []
            src = dram.ap().rearrange("(k p) n -> p k n", p=128)
            for q in range(4):
                r = Res("%s_%d" % (name, q))
                P.dma("pool", DMA(W[:, q * 4:(q + 1) * 4, :], src[:, q * 4:(q + 1) * 4, :]), P.chan(), writes=[r])
                parts.append(r)
            return W, parts

        def fm_group(W, Wr, c0, M, hT, bank):
            P.op("pe", [MM(bank[0:M, 0:512], W[:, k, c0:c0 + M], hT[:, k, :], k == 0, k == 15) for k in range(16)],
                 reads=Wr + [hT.r], writes=[bank.r])

        def tm_group(W, Wr, c0, N, hT, tt, bank):
            P.op("pe", [MM(bank[:, 0:N], hT[:, k, tt * 128:(tt + 1) * 128], W[:, k, c0:c0 + N], k == 0, k == 15)
                        for k in range(16)], reads=Wr + [hT.r], writes=[bank.r])

        load_wn(0)
        if stop_after == 0:
            P.emit()
            return nc, P

        ph = ExitStack(); P.alloc = ph
        Ws, Wsr = load_w(ws_in_d, 1544, "Ws")
        hTs = [P.sb("hT%d" % i, [128, 16, 512], BF16) for i in range(2)]
        xraw = [P.sb("xraw%d" % c, [128, 515], F32) for c in range(8)]
        acc = [P.sb("acc%d" % i, [128, 512], F32) for i in range(2)]
        xcT = [[P.sb("xcT%d_%d" % (i, c), [128, 512], BF16) for c in range(8)] for i in range(2)]
        zs = [P.sb("zs%d" % i, [128, 512], BF16) for i in range(8)]
        dtv = [P.sb("dtv%d" % i, [128, 32], F32) for i in range(8)]
        ed = [P.sb("ed%d" % i, [128, 16], F32) for i in range(2)]
        na = [P.sb("na%d" % i, [128, 8], F32) for i in range(2)]
        acs = [P.sb("acs%d" % i, [128, 8], F32) for i in range(2)]
        acsT = [P.sb("acsT%d" % i, [8, 128], F32) for i in range(2)]
        sel8 = P.sb("sel8", [8, 1024], F32)
        ea = P.sb("ea", [128, 8], F32)
        eAl = [P.sb("eAl%d" % i, [128, 8], F32) for i in range(2)]
        Lh = [P.sb("Lh%d" % i, [128, 128], F32) for i in range(3)]
        Lcl = [P.sb("Lcl%d" % i, [128, 128], F32) for i in range(3)]
        MT = [P.sb("MT%d" % i, [128, 128], BF16) for i in range(4)]
        cbs = [P.sb("cbs%d" % i, [128, 256], F32) for i in range(2)]
        xtm = [P.sb("xtm%d" % i, [128, 768], BF16) for i in range(2)]
        xsdt = [P.sb("xsdt%d" % i, [128, 512], BF16) for i in range(2)]
        xsdec = [P.sb("xsdec%d" % i, [128, 512], BF16) for i in range(2)]
        ysb = [P.sb("ysb%d" % i, [128, 512], F32) for i in range(2)]
        ytmp = P.sb("ytmp", [128, 512], F32)
        RT = P.sb("RT", [128, 512], F32)
        RTb = P.sb("RTb", [128, 512], BF16)
        nss = [P.sb("nss%d" % i, [128, 8], F32) for i in range(2)]
        ob = [P.sb("ob%d" % i, [128, 512], BF16) for i in range(2)]
        ch_ob = [P.chan(), P.chan()]

        P.dma("sp", DMA(sel8[:], sel_d.ap()), P.chan(), writes=[sel8.r])
        P.op("act", ACT(ea[:], alog, AF.Exp), reads=[smalls.r], writes=[ea.r])
        P.op("dve", MSET(RT[:], 0.0), writes=[RT.r])
        P.op("dve", MSET(RTb[:], 0.0), writes=[RTb.r])
        for c in range(8):
            P.op("dve", MSET(xraw[c][:, 0:3], 0.0), writes=[xraw[c].r])

        def front_tile(blk, tt, hT):
            t = blk * 4 + tt
            s = t % 2
            P.dma("sp", DMA(xt[s][:], x_full[t * 128:(t + 1) * 128, :]), ch_x[s], writes=[xt[s].r])
            rmsnorm_to_bf16(xt[s], s, hb[s])
            transpose16(hb[s], hT, tt * 128)

        def ssd_fm(blk, c):
            hT = hTs[blk % 2]
            xc = xcT[blk % 2][c]
            bank = P.bank()
            fm_group(Ws, Wsr, c * 128, 128, hT, bank)
            P.op("act", ACP(xraw[c][:, 3:515], bank[:, 0:512]), reads=[bank.r], writes=[xraw[c].r])
            a = acc[c % 2]
            P.op("dve", TS(a[:], xraw[c][:, 3:515], wc[:, c * 4 + 3:c * 4 + 4], bcv[:, c:c + 1], ALU.mult, ALU.add),
                 reads=[xraw[c].r, smalls.r], writes=[a.r])
            for k in (2, 1, 0):
                P.op("dve", STT(a[:], xraw[c][:, k:k + 512], wc[:, c * 4 + k:c * 4 + k + 1], a[:], ALU.mult, ALU.add),
                     reads=[xraw[c].r, smalls.r, a.r], writes=[a.r])
            P.op("act", ACT(xc[:], a[:], AF.Silu), reads=[a.r], writes=[xc.r])
            P.op("dve", CP(xraw[c][:, 0:3], xraw[c][:, 512:515]), reads=[xraw[c].r], writes=[xraw[c].r])

        def ssd_tm(blk, tt):
            hT = hTs[blk % 2]
            z_ = zs[(blk % 2) * 4 + tt]
            d_ = dtv[(blk % 2) * 4 + tt]
            bank = P.bank()
            tm_group(Ws, Wsr, 1024, 512, hT, tt, bank)
            P.op("act", ACT(z_[:], bank[:, 0:512], AF.Silu), reads=[bank.r], writes=[z_.r])
            bank = P.bank()
            tm_group(Ws, Wsr, 1536, 8, hT, tt, bank)
            P.op("dve", TT(d_[:, 0:8], bank[:, 0:8], dtb, ALU.add), reads=[bank.r, smalls.r], writes=[d_.r])
            P.op("act", ACT(d_[:, 8:16], d_[:, 0:8], AF.Exp), reads=[d_.r], writes=[d_.r])
            P.op("act", ACT(d_[:, 16:24], d_[:, 8:16], AF.Ln, bias=1.0), reads=[d_.r], writes=[d_.r])
            P.op("dve", STT(d_[:, 24:32], d_[:, 16:24], -1.0, ea[:], ALU.mult, ALU.mult),
                 reads=[d_.r, ea.r], writes=[d_.r])

        def ssd_s2(blk, tt):
            t = blk * 4 + tt
            s = t % 2
            tc_ = slice(tt * 128, (tt + 1) * 128)
            xcT_ = xcT[blk % 2]
            z_ = zs[(blk % 2) * 4 + tt]
            d_ = dtv[(blk % 2) * 4 + tt]
            dt_ = d_[:, 16:24]; dA = d_[:, 24:32]
            bank = P.bank()
            P.op("pe", [MM(bank[:, 0:8], TriInc, dA), MM(bank[:, 8:16], TriGt, dA)],
                 reads=[cf.r, d_.r], writes=[bank.r])
            P.op("act", ACT(ed[s][:], bank[:, 0:16], AF.Exp), reads=[bank.r], writes=[ed[s].r])
            P.op("dve", TS(na[s][:], bank[:, 0:8], -1.0, None, ALU.mult), reads=[bank.r], writes=[na[s].r])
            P.op("act", ACP(acs[s][:], bank[:, 0:8]), reads=[bank.r], writes=[acs[s].r])
            bank = P.bank()
            P.op("pe", MM(bank[0:8, 0:128], acs[s][:], IdentF), reads=[acs[s].r, cf.r], writes=[bank.r])
            P.op("act", ACP(acsT[s][:], bank[0:8, 0:128]), reads=[bank.r], writes=[acsT[s].r])
            bank = P.bank()
            P.op("pe", [MM(bank[:, gi * 128:(gi + 1) * 128], xcT_[4 + gi][:, tc_], xcT_[6 + gi][:, tc_]) for gi in range(2)],
                 reads=[xcT_[4].r, xcT_[5].r, xcT_[6].r, xcT_[7].r], writes=[bank.r])
            P.op("dve", TT(v3(cbs[s][:, :], 128), v3(bank[:, 0:256], 128), TriInc.unsqueeze(1).broadcast_to([128, 2, 128]), ALU.mult),
                 reads=[bank.r, cf.r], writes=[cbs[s].r])
            tp = P.tpbank()
            P.op("pe", [TR(tp[:, c * 128:(c + 1) * 128], xcT_[c][:, tc_], ident) for c in range(6)],
                 reads=[xcT_[c].r for c in range(6)] + [cbf.r], writes=[tp.r])
            P.op("act", ACP(xtm[s][:], tp[:, 0:768]), reads=[tp.r], writes=[xtm[s].r])
            P.op("dve", TT(v3(xsdt[s][:, :], 64), v3(xtm[s][:, 0:512], 64), bc3(dt_, 64), ALU.mult),
                 reads=[xtm[s].r, d_.r], writes=[xsdt[s].r])
            P.op("dve", TT(v3(xsdec[s][:, :], 64), v3(xsdt[s][:, :], 64), bc3(ed[s][:, 8:16], 64), ALU.mult),
                 reads=[xsdt[s].r, ed[s].r], writes=[xsdec[s].r])
            bankY = P.bank(); bankO = P.bank(); bankS = P.bank()
            ywr = []
            for hq in range(2):
                bankA = P.bank()
                fns = []
                for h4 in range(4):
                    h = hq * 4 + h4
                    fns.append(MM(bankA[:, h4 * 128:(h4 + 1) * 128], sel8[:, h * 128:(h + 1) * 128], acsT[s][:], True, True))
                P.op("pe", fns, reads=[sel8.r, acsT[s].r, cf.r], writes=[bankA.r])
                P.op("act", ACT(eAl[s][:, hq * 4:(hq + 1) * 4], v3(bankA[:, :], 128)[:, :, 127], AF.Exp),
                     reads=[bankA.r], writes=[eAl[s].r])
                for h4 in range(4):
                    h = hq * 4 + h4
                    gi = h // 4
                    L = Lh[h % 3]; M_ = MT[h % 4]
                    Lc = Lcl[h % 3]
                    P.op("dve", TS(Lc[:], bankA[:, h4 * 128:(h4 + 1) * 128], na[s][:, h:h + 1], 0.0, ALU.add, ALU.min),
                         reads=[bankA.r, na[s].r], writes=[Lc.r])
                    P.op("act", ACT(L[:], Lc[:], AF.Exp), reads=[Lc.r], writes=[L.r])
                    P.op("dve", TT(M_[:], L[:], cbs[s][:, gi * 128:(gi + 1) * 128], ALU.mult),
                         reads=[L.r, cbs[s].r], writes=[M_.r])
                    hc = slice(h * 64, (h + 1) * 64)
                    P.op("pe", [MM(bankY[:, hc], M_[:], xsdt[s][:, hc]),
                                MM(bankO[:, hc], xcT_[6 + gi][:, tc_], RTb[:, hc]),
                                MM(bankS[:, hc], xtm[s][:, 512 + gi * 128:512 + (gi + 1) * 128], xsdec[s][:, hc])],
                         reads=[M_.r, xsdt[s].r, xcT_[6 + gi].r, RTb.r, xtm[s].r, xsdec[s].r],
                         writes=[bankY.r, bankO.r, bankS.r])
            y_ = ysb[s]
            P.op("dve", TT(v3(y_[:, :], 64), v3(bankO[:, 0:512], 64), bc3(ed[s][:, 0:8], 64), ALU.mult),
                 reads=[bankO.r, ed[s].r], writes=[y_.r])
            P.op("dve", TT(y_[:], y_[:], bankY[:, 0:512], ALU.add), reads=[y_.r, bankY.r], writes=[y_.r])
            P.op("dve", TT(v3(ytmp[:, :], 64), v3(xtm[s][:, 0:512], 64), bc3(dsk, 64), ALU.mult),
                 reads=[xtm[s].r, smalls.r], writes=[ytmp.r])
            P.op("dve", TT(y_[:], y_[:], ytmp[:], ALU.add), reads=[y_.r, ytmp.r], writes=[y_.r])
            P.op("dve", TT(y_[:], y_[:], z_[:], ALU.mult), reads=[y_.r, z_.r], writes=[y_.r])
            P.op("dve", TT(v3(RT[:, :], 64), v3(RT[:, :], 64), bc3(eAl[s][:, :], 64), ALU.mult),
                 reads=[RT.r, eAl[s].r], writes=[RT.r])
            P.op("dve", TT(RT[:], RT[:], bankS[:, 0:512], ALU.add), reads=[RT.r, bankS.r], writes=[RT.r])
            P.op("act", ACP(RTb[:], RT[:]), reads=[RT.r], writes=[RTb.r])
            n_ = nss[s]
            P.op("dve", MSET(n_[:, 0:2], 0.0), writes=[n_.r])
            for gi in range(2):
                P.op("act", ACT(junk[:, 0:256], y_[:, gi * 256:(gi + 1) * 256], AF.Square, scale=1.0 / 16.0,
                                accum_out=n_[:, gi:gi + 1]), reads=[y_.r], writes=[junk.r, n_.r])
            P.op("act", ACT(n_[:, 2:4], n_[:, 0:2], AF.Sqrt, bias=EPS), reads=[n_.r], writes=[n_.r])
            P.op("dve", RECIP(n_[:, 4:6], n_[:, 2:4]), reads=[n_.r], writes=[n_.r])
            for gi in range(2):
                P.op("dve", STT(ob[s][:, gi * 256:(gi + 1) * 256], y_[:, gi * 256:(gi + 1) * 256], n_[:, 4 + gi:5 + gi],
                                wsn[:, gi * 256:(gi + 1) * 256], ALU.mult, ALU.mult),
                     reads=[y_.r, n_.r, smalls.r], writes=[ob[s].r])
            P.dma("sp", DMA(mixbuf[t * 128:(t + 1) * 128, 512:1024], ob[s][:]), ch_ob[s], reads=[ob[s].r], accw=[R_mix[blk]])

        for tt in range(4):
            front_tile(0, tt, hTs[0])
        for c in range(8):
            ssd_fm(0, c)
        for tt in range(4):
            ssd_tm(0, tt)
        for tt in range(4):
            front_tile(1, tt, hTs[1])
        for blk in range(8):
            for tt in range(4):
                if blk + 1 < 8:
                    ssd_fm(blk + 1, 2 * tt)
                    ssd_fm(blk + 1, 2 * tt + 1)
                    ssd_tm(blk + 1, tt)
                if blk + 2 < 8:
                    front_tile(blk + 2, tt, hTs[blk % 2])
                ssd_s2(blk, tt)
        P.barrier()
        if stop_after == 1:
            if dbg:
                chd = P.chan()
                for i_ in range(32):
                    P.dma("sp", DMA(dbg_mix[i_ * 128:(i_ + 1) * 128, :], mixbuf[i_ * 128:(i_ + 1) * 128, :]), chd, reads=R_mix)
            P.emit()
            ph.close()
            return nc, P
        ph.close()

        ph = ExitStack(); P.alloc = ph
        Wg, Wgr = load_w(wg_in_d, 1808, "Wg")
        hTs = [P.sb("hT%d" % i, [128, 16, 512], BF16) for i in range(2)]
        qT = [P.sb("qT%d" % i, [128, 2, 512], F32) for i in range(2)]
        kT = [P.sb("kT%d" % i, [128, 2, 512], F32) for i in range(2)]
        gkT = [P.sb("gkT%d" % i, [33, 512], BF16) for i in range(2)]
        wgk_f = P.sb("wgk_f", [33, 256], F32)
        wgk = P.sb("wgk", [33, 256], BF16)
        vb = [P.sb("vb%d" % i, [128, 512], BF16) for i in range(8)]
        gs = [P.sb("gs%d" % i, [128, 512], BF16) for i in range(8)]
        ktm = [P.sb("ktm%d" % i, [128, 256], BF16) for i in range(8)]
        sp1 = [P.sb("sp1%d" % i, [128, 256], F32) for i in range(2)]
        spl = [P.sb("spl%d" % i, [128, 256], F32) for i in range(2)]
        eG = [P.sb("eG%d" % i, [128, 256], F32) for i in range(2)]
        eGn = [P.sb("eGn%d" % i, [128, 256], F32) for i in range(2)]
        eD2 = [P.sb("eD2%d" % i, [128, 256], F32) for i in range(2)]
        qtf = [P.sb("qtf%d" % i, [128, 256], BF16) for i in range(2)]
        ktf = [P.sb("ktf%d" % i, [128, 256], BF16) for i in range(2)]
        khat = [P.sb("khat%d" % i, [128, 256], BF16) for i in range(2)]
        qlo = [P.sb("qlo%d" % i, [128, 256], BF16) for i in range(2)]
        qhi = [P.sb("qhi%d" % i, [128, 256], BF16) for i in range(2)]
        Sm = [P.sb("Sm%d" % i, [128, 256], BF16) for i in range(2)]
        Sf = P.sb("Sf", [128, 512], F32); Sb = P.sb("Sb", [128, 512], BF16)
        Sfm = P.sb("Sfm", [128, 512], F32); Sbm = P.sb("Sbm", [128, 512], BF16)
        otmp = [P.sb("otmp%d" % i, [128, 512], F32) for i in range(2)]
        nss = [P.sb("nss%d" % i, [128, 8], F32) for i in range(2)]
        ob = [P.sb("ob%d" % i, [128, 512], BF16) for i in range(2)]
        ch_ob = [P.chan(), P.chan()]

        P.dma("sp", DMA(wgk_f[:], wgk_d.ap()), P.chan(), writes=[wgk_f.r])
        P.op("dve", CP(wgk[:], wgk_f[:]), reads=[wgk_f.r], writes=[wgk.r])
        for i in range(2):
            P.op("dve", MSET(gkT[i][0:32, :], 0.0), writes=[gkT[i].r])
            P.op("dve", MSET(gkT[i][32:33, :], 1.0), writes=[gkT[i].r])
        P.op("dve", MSET(Sf[:], 0.0), writes=[Sf.r])
        P.op("dve", MSET(Sb[:], 0.0), writes=[Sb.r])
        for i in range(2):
            P.op("dve", MSET(qlo[i][:], 0.0), writes=[qlo[i].r])
            P.op("dve", MSET(qhi[i][:], 0.0), writes=[qhi[i].r])

        def gla_fm(blk, which):
            hT = hTs[blk % 2]
            qT_ = qT[blk % 2]; kT_ = kT[blk % 2]; gkT_ = gkT[blk % 2]
            bank = P.bank()
            if which < 2:
                hh = which
                fm_group(Wg, Wgr, hh * 128, 128, hT, bank)
                P.op("act", AMUL(qT_[:, hh, :], bank[:, 0:512], float(128.0 ** -0.5)), reads=[bank.r], writes=[qT_.r])
            elif which < 4:
                hh = which - 2
                fm_group(Wg, Wgr, 256 + hh * 128, 128, hT, bank)
                P.op("act", ACP(kT_[:, hh, :], bank[:, 0:512]), reads=[bank.r], writes=[kT_.r])
            else:
                fm_group(Wg, Wgr, 512, 16, hT, bank)
                P.op("act", ACP(gkT_[0:16, :], bank[0:16, 0:512]), reads=[bank.r], writes=[gkT_.r])

        def gla_tm(blk, tt):
            hT = hTs[blk % 2]
            i_ = (blk % 2) * 4 + tt
            bank = P.bank()
            tm_group(Wg, Wgr, 528, 512, hT, tt, bank)
            P.op("act", ACP(vb[i_][:], bank[:, 0:512]), reads=[bank.r], writes=[vb[i_].r])
            bank = P.bank()
            tm_group(Wg, Wgr, 1040, 512, hT, tt, bank)
            P.op("act", ACT(gs[i_][:], bank[:, 0:512], AF.Silu), reads=[bank.r], writes=[gs[i_].r])
            bank = P.bank()
            tm_group(Wg, Wgr, 1552, 256, hT, tt, bank)
            P.op("act", ACP(ktm[i_][:], bank[:, 0:256]), reads=[bank.r], writes=[ktm[i_].r])

        def gla_s2(blk, tt):
            t = blk * 4 + tt
            s = t % 2
            tc_ = slice(tt * 128, (tt + 1) * 128)
            qT_ = qT[blk % 2]; kT_ = kT[blk % 2]; gkT_ = gkT[blk % 2]
            i_ = (blk % 2) * 4 + tt
            vb_ = vb[i_]; gs_ = gs[i_]; ktm_ = ktm[i_]
            bank = P.bank()
            P.op("pe", MM(bank[:, 0:256], gkT_[0:33, tc_], wgk[:]), reads=[gkT_.r, wgk.r], writes=[bank.r])
            P.op("act", ACT(sp1[s][:], bank[:, 0:256], AF.Exp, scale=-1.0), reads=[bank.r], writes=[sp1[s].r])
            P.op("act", ACT(spl[s][:], sp1[s][:], AF.Ln, bias=1.0), reads=[sp1[s].r], writes=[spl[s].r])
            bank = P.bank()
            P.op("pe", [MM(bank[:, hh * 128:(hh + 1) * 128], spl[s][:, hh * 128:(hh + 1) * 128], TriIncBD) for hh in range(2)],
                 reads=[spl[s].r, cf.r], writes=[bank.r])
            P.op("act", ACT(eG[s][:], bank[:, 0:256], AF.Exp), reads=[bank.r], writes=[eG[s].r])
            P.op("act", ACT(eGn[s][:], bank[:, 0:256], AF.Exp, scale=-1.0), reads=[bank.r], writes=[eGn[s].r])
            bank = P.bank()
            P.op("pe", MM(bank[:, 0:256], TriGtBD, spl[s][:]), reads=[spl[s].r, cf.r], writes=[bank.r])
            P.op("act", ACT(eD2[s][:], bank[:, 0:256], AF.Exp), reads=[bank.r], writes=[eD2[s].r])
            P.op("dve", TT(v3(qtf[s][:, :], 128), qT_[:, :, tc_], v3(eG[s][:, :], 128), ALU.mult),
                 reads=[qT_.r, eG[s].r], writes=[qtf[s].r])
            P.op("dve", TT(v3(ktf[s][:, :], 128), kT_[:, :, tc_], v3(eGn[s][:, :], 128), ALU.mult),
                 reads=[kT_.r, eGn[s].r], writes=[ktf[s].r])
            P.op("dve", TT(khat[s][:], ktm_[:], eD2[s][:], ALU.mult), reads=[ktm_.r, eD2[s].r], writes=[khat[s].r])
            P.op("act", ACP(v3(qlo[s][:, :], 128)[:, :, 0:64], v3(qtf[s][:, :], 128)[:, :, 0:64]),
                 reads=[qtf[s].r], writes=[qlo[s].r])
            P.op("act", ACP(v3(qhi[s][:, :], 128)[:, :, 64:128], v3(qtf[s][:, :], 128)[:, :, 64:128]),
                 reads=[qtf[s].r], writes=[qhi[s].r])
            bank = P.bank()
            P.op("pe", [MM(bank[:, hh * 128:(hh + 1) * 128], ktf[s][:, hh * 128:(hh + 1) * 128], qtf[s][:, hh * 128:(hh + 1) * 128])
                        for hh in range(2)], reads=[ktf[s].r, qtf[s].r], writes=[bank.r])
            P.op("dve", TT(Sm[s][:], bank[:, 0:256], MaskBD2, ALU.mult), reads=[bank.r, cf.r], writes=[Sm[s].r])
            bankT = P.bank()
            P.op("pe", [MM(bankT[:, hh * 256:(hh + 1) * 256], khat[s][0:64, hh * 128:(hh + 1) * 128], vb_[0:64, hh * 256:(hh + 1) * 256])
                        for hh in range(2)], reads=[khat[s].r, vb_.r], writes=[bankT.r])
            for hh in range(2):
                P.op("dve", STT(Sfm[:, hh * 256:(hh + 1) * 256], Sf[:, hh * 256:(hh + 1) * 256],
                                eG[s][:, hh * 128 + 63:hh * 128 + 64], bankT[:, hh * 256:(hh + 1) * 256], ALU.mult, ALU.add),
                     reads=[Sf.r, eG[s].r, bankT.r], writes=[Sfm.r])
            P.op("act", ACP(Sbm[:], Sfm[:]), reads=[Sfm.r], writes=[Sbm.r])
            bankO = P.bank()
            fns = []
            for hh in range(2):
                oc = slice(hh * 256, (hh + 1) * 256); hc = slice(hh * 128, (hh + 1) * 128)
                fns.append(MM(bankO[:, oc], Sm[s][:, hc], vb_[:, oc], True, False))
                fns.append(MM(bankO[:, oc], qlo[s][:, hc], Sb[:, oc], False, False))
                fns.append(MM(bankO[:, oc], qhi[s][:, hc], Sbm[:, oc], False, True))
            P.op("pe", fns, reads=[Sm[s].r, vb_.r, qlo[s].r, qhi[s].r, Sb.r, Sbm.r], writes=[bankO.r])
            bankT = P.bank()
            P.op("pe", [MM(bankT[:, hh * 256:(hh + 1) * 256], khat[s][64:128, hh * 128:(hh + 1) * 128], vb_[64:128, hh * 256:(hh + 1) * 256])
                        for hh in range(2)], reads=[khat[s].r, vb_.r], writes=[bankT.r])
            for hh in range(2):
                P.op("dve", STT(Sf[:, hh * 256:(hh + 1) * 256], Sfm[:, hh * 256:(hh + 1) * 256],
                                eG[s][:, hh * 128 + 127:hh * 128 + 128], bankT[:, hh * 256:(hh + 1) * 256], ALU.mult, ALU.add),
                     reads=[Sfm.r, eG[s].r, bankT.r], writes=[Sf.r])
            P.op("act", ACP(Sb[:], Sf[:]), reads=[Sf.r], writes=[Sb.r])
            n_ = nss[s]
            P.op("dve", MSET(n_[:, 0:2], 0.0), writes=[n_.r])
            for hh in range(2):
                P.op("act", ACT(junk[:, 0:256], bankO[:, hh * 256:(hh + 1) * 256], AF.Square, scale=1.0 / 16.0,
                                accum_out=n_[:, hh:hh + 1]), reads=[bankO.r], writes=[junk.r, n_.r])
            P.op("act", ACT(n_[:, 2:4], n_[:, 0:2], AF.Sqrt, bias=EPS), reads=[n_.r], writes=[n_.r])
            P.op("dve", RECIP(n_[:, 4:6], n_[:, 2:4]), reads=[n_.r], writes=[n_.r])
            for hh in range(2):
                P.op("dve", STT(otmp[s][:, hh * 256:(hh + 1) * 256], bankO[:, hh * 256:(hh + 1) * 256], n_[:, 4 + hh:5 + hh],
                                wgn, ALU.mult, ALU.mult), reads=[bankO.r, n_.r, smalls.r], writes=[otmp[s].r])
            P.op("dve", TT(ob[s][:], otmp[s][:], gs_[:], ALU.mult), reads=[otmp[s].r, gs_.r], writes=[ob[s].r])
            P.dma("sp", DMA(mixbuf[t * 128:(t + 1) * 128, 0:512], ob[s][:]), ch_ob[s], reads=[ob[s].r], accw=[R_mix[blk]])

        for tt in range(4):
            front_tile(0, tt, hTs[0])
        for w_ in range(5):
            gla_fm(0, w_)
        for tt in range(4):
            gla_tm(0, tt)
        for tt in range(4):
            front_tile(1, tt, hTs[1])
        for blk in range(8):
            for tt in range(4):
                if blk + 1 < 8:
                    for w_ in ((0, 1), (2, 3), (4,), ())[tt]:
                        gla_fm(blk + 1, w_)
                    gla_tm(blk + 1, tt)
                if blk + 2 < 8:
                    front_tile(blk + 2, tt, hTs[blk % 2])
                gla_s2(blk, tt)
            P.collective(lambda e, blk=blk: e.collective_compute("AllGather", ALU.bypass,
                                                                 replica_groups=[[0, 1], [2, 3], [4, 5], [6, 7]],
                                                                 ins=[mixbuf[blk * 512:(blk + 1) * 512, :].opt()],
                                                                 outs=[gath[blk * 1024:(blk + 1) * 1024, :].opt()]),
                         reads=[R_mix[blk]], writes=[R_gath[blk]])
        P.barrier()
        if stop_after == 2:
            if dbg:
                chd = P.chan()
                for i_ in range(32):
                    P.dma("sp", DMA(dbg_mix[i_ * 128:(i_ + 1) * 128, :], mixbuf[i_ * 128:(i_ + 1) * 128, :]), chd, reads=R_mix)
            P.emit()
            ph.close()
            return nc, P
        ph.close()

        if dbg:
            chd = P.chan()
            for i_ in range(32):
                P.dma("sp", DMA(dbg_mix[i_ * 128:(i_ + 1) * 128, :], mixbuf[i_ * 128:(i_ + 1) * 128, :]), chd, reads=R_mix)
        if stop_after == 21:
            P.barrier()
            P.emit()
            return nc, P
        ph = ExitStack(); P.alloc = ph
        Wo, Wor = load_w(wout_d, 2048, "Wo")
        Wr, Wrr = load_w(wr_d, 36, "Wr")
        ridx = P.sb("ridx", [128, 32], I32)
        mt = [P.sb("mt%d" % i, [128, 2048], BF16) for i in range(2)]
        mixT = [P.sb("mixT%d" % i, [128, 16, 128], BF16) for i in range(2)]
        x1 = [P.sb("x1_%d" % i, [128, D], F32) for i in range(2)]
        h2T = [P.sb("h2T%d" % i, [128, 16, 128], BF16) for i in range(2)]
        rt = [P.sb("rt%d" % i, [128, 256], F32) for i in range(2)]
        Ab = [P.sb("Ab%d" % i, [128, 32], BF16) for i in range(2)]
        cntb = P.sb("cntb", [128, 32], F32)
        slots_f = P.sb("slots_f", [128, 32], F32)
        ch_mt = [[P.chan(), P.chan()], [P.chan(), P.chan()]]
        ch_x1 = [P.chan(), P.chan()]
        ch_sc = [[P.chan(), P.chan()], [P.chan(), P.chan()]]
        P.dma("sp", DMA(ridx[:], ridx_d.ap()), P.chan(), writes=[ridx.r])
        P.op("dve", MSET(cntb[:], 0.0), writes=[cntb.r])
        load_wn(1)
        def b_s1a(t):
            s = t % 2
            rmt = [Res("mt%d_%d" % (s, r)) for r in range(2)]
            for r in range(2):
                P.dma("pool", GATHER(mt[s][:, r * 1024:(r + 1) * 1024], gath[:, :], ridx[:, 2 * t + r:2 * t + r + 1], 2 * SEQ - 1),
                      ch_mt[s][r], reads=[R_gath[t // 4], R_gath[4 + t // 4], ridx.r], writes=[mt[s].r] if r == 0 else [], accw=[] if r == 0 else [mt[s].r])
            P.dma("sp", DMA(xt[s][:], x_half[t * 128:(t + 1) * 128, :]), ch_x[s], writes=[xt[s].r])
            transpose16(mt[s], mixT[s], 0)
            obanks = []
            for db in range(4):
                bank = P.bank()
                obanks.append(bank)
                P.op("pe", [MM(bank[:, 0:512], mixT[s][:, k, :], Wo[:, k, db * 512:(db + 1) * 512], k == 0, k == 15) for k in range(16)],
                     reads=Wor + [mixT[s].r], writes=[bank.r])
            return obanks

        def b_s1b(t, obanks):
            s = t % 2
            for db in range(4):
                bank = obanks[db]
                P.op("dve", TT(x1[s][:, db * 512:(db + 1) * 512], bank[:, 0:512], xt[s][:, db * 512:(db + 1) * 512], ALU.add),
                     reads=[bank.r, xt[s].r], writes=[x1[s].r])
            P.dma("sp", DMA(x1buf[t * 128:(t + 1) * 128, :], x1[s][:]), ch_x1[s], reads=[x1[s].r], accw=[R_x1])
            if dbg:
                P.dma("sp", DMA(dbg_x1[t * 128:(t + 1) * 128, :], x1[s][:]), ch_x1[s], reads=[x1[s].r])
            rmsnorm_to_bf16(x1[s], s, hb[s])
            transpose16(hb[s], h2T[s], 0)
            bank = P.bank()
            P.op("pe", [MM(bank[:, 0:36], h2T[s][:, k, :], Wr[:, k, 0:36], k == 0, k == 15) for k in range(16)],
                 reads=Wrr + [h2T[s].r], writes=[bank.r])
            q = rt[s]; qr = [q.r]
            L = q[:, 0:36]
            P.op("dve", TT(L, bank[:, 0:36], brb, ALU.add), reads=[bank.r, smalls.r], writes=qr)

        def b_s2(t):
            s = t % 2
            q = rt[s]; qr = [q.r]
            L = q[:, 0:36]
            gmax = q[:, 36:37]; ngmax = q[:, 37:38]; gsum = q[:, 38:39]; gw = q[:, 39:40]
            gexp = q[:, 40:44]; ohg = q[:, 44:48]; esel = q[:, 48:56]; m1 = q[:, 56:57]; oh1 = q[:, 57:65]
            esel2 = q[:, 65:73]; m2 = q[:, 73:74]; oh2 = q[:, 74:82]; dd = q[:, 82:83]; e2 = q[:, 83:84]
            rden = q[:, 84:85]; OH1 = q[:, 88:120]; OH2 = q[:, 120:152]; pos = q[:, 152:184]; tmpa = q[:, 184:216]
            psel = q[:, 216:218]; ssum = q[:, 218:220]; ovf = q[:, 220:222]; Af = q[:, 222:254]
            P.op("dve", RMAX(gmax, L[:, 0:4]), reads=qr, writes=qr)
            P.op("dve", TS(ngmax, gmax, -1.0, None, ALU.mult), reads=qr, writes=qr)
            P.op("dve", MSET(gsum, 0.0), reads=qr, writes=qr)
            P.op("act", ACT(gexp, L[:, 0:4], AF.Exp, bias=ngmax, accum_out=gsum), reads=qr, writes=qr)
            P.op("dve", RECIP(gw, gsum), reads=qr, writes=qr)
            P.op("dve", TS(ohg, L[:, 0:4], gmax, None, ALU.is_equal), reads=qr, writes=qr)
            P.op("dve", TS(esel, L[:, 4:12], ohg[:, 0:1], None, ALU.mult), reads=qr, writes=qr)
            for g_ in range(1, 4):
                P.op("dve", STT(esel, L[:, 4 + 8 * g_:12 + 8 * g_], ohg[:, g_:g_ + 1], esel, ALU.mult, ALU.add), reads=qr, writes=qr)
            P.op("dve", RMAX(m1, esel), reads=qr, writes=qr)
            P.op("dve", TS(oh1, esel, m1, None, ALU.is_equal), reads=qr, writes=qr)
            P.op("dve", STT(esel2, oh1, -1e30, esel, ALU.mult, ALU.add), reads=qr, writes=qr)
            P.op("dve", RMAX(m2, esel2), reads=qr, writes=qr)
            P.op("dve", TS(oh2, esel2, m2, None, ALU.is_equal), reads=qr, writes=qr)
            P.op("dve", TT(dd, m2, m1, ALU.subtract), reads=qr, writes=qr)
            P.op("act", ACT(e2, dd, AF.Exp), reads=qr, writes=qr)
            P.op("dve", TS(rden, e2, 1.0, None, ALU.add), reads=qr, writes=qr)
            P.op("dve", RECIP(rden, rden), reads=qr, writes=qr)
            P.op("dve", TT(cw[:, 2 * t:2 * t + 1], gw, rden, ALU.mult), reads=qr, writes=[cw.r])
            P.op("dve", TT(cw[:, 2 * t + 1:2 * t + 2], cw[:, 2 * t:2 * t + 1], e2, ALU.mult), reads=qr + [cw.r], writes=[cw.r])
            for g_ in range(4):
                P.op("dve", TS(OH1[:, 8 * g_:8 * g_ + 8], oh1, ohg[:, g_:g_ + 1], None, ALU.mult), reads=qr, writes=qr)
                P.op("dve", TS(OH2[:, 8 * g_:8 * g_ + 8], oh2, ohg[:, g_:g_ + 1], None, ALU.mult), reads=qr, writes=qr)
            P.op("dve", TT(Af, OH1, OH2, ALU.add), reads=qr, writes=qr)
            P.op("dve", CP(Ab[s][:], Af), reads=qr, writes=[Ab[s].r])
            bank = P.bank()
            P.op("pe", [MM(bank[:, 0:32], TriLt_b, Ab[s][:]), MM(bank[:, 32:64], ones_b, Ab[s][:])],
                 reads=[cbf.r, Ab[s].r], writes=[bank.r])
            P.op("dve", TT(pos, bank[:, 0:32], cntb[:], ALU.add), reads=[bank.r, cntb.r], writes=qr)
            P.op("dve", TT(cntb[:], cntb[:], bank[:, 32:64], ALU.add), reads=[bank.r, cntb.r], writes=[cntb.r])
            for j, OH in enumerate((OH1, OH2)):
                P.op("dve", TT(tmpa, pos, OH, ALU.mult), reads=qr, writes=qr)
                P.op("dve", RSUM(psel[:, j:j + 1], tmpa), reads=qr, writes=qr)
                P.op("dve", TT(tmpa, eC, OH, ALU.mult), reads=qr + [cf.r], writes=qr)
                P.op("dve", RSUM(ssum[:, j:j + 1], tmpa), reads=qr, writes=qr)
            P.op("dve", TS(ovf, psel, float(CAP) - 0.5, 1.0e6, ALU.is_ge, ALU.mult), reads=qr, writes=qr)
            P.op("dve", TT(ssum, ssum, psel, ALU.add), reads=qr, writes=qr)
            P.op("dve", TT(slots_f[:, 2 * t:2 * t + 2], ssum, ovf, ALU.add), reads=qr, writes=[slots_f.r])
            P.op("dve", CP(slots_i[:, 2 * t:2 * t + 2], slots_f[:, 2 * t:2 * t + 2]), reads=[slots_f.r], writes=[slots_i.r])
            for j in range(2):
                P.dma("pool", SCATTER(hs[:, :], slots_i[:, 2 * t + j:2 * t + j + 1], hb[s][:, :], NSLOT - 1),
                      ch_sc[s][j], reads=[hb[s].r, slots_i.r], accw=[R_hs])

        ob_ = b_s1a(0)
        b_s1b(0, ob_)
        for t in range(NT_HALF):
            if t + 1 < NT_HALF:
                ob_ = b_s1a(t + 1)
            b_s2(t)
            if t + 1 < NT_HALF:
                b_s1b(t + 1, ob_)
        if dbg:
            P.dma("sp", DMA(dbg_slots.ap(), slots_i[:]), P.chan(), reads=[slots_i.r])
            P.dma("sp", DMA(dbg_cw.ap(), cw[:]), P.chan(), reads=[cw.r])
        P.barrier()
        if stop_after == 3:
            P.emit()
            ph.close()
            return nc, P
        ph.close()

        ph = ExitStack(); P.alloc = ph
        wgt = [P.sb("wgt%d" % i, [128, 16, 512], BF16) for i in range(2)]
        wut = [P.sb("wut%d" % i, [128, 16, 512], BF16) for i in range(2)]
        wdt = [P.sb("wdt%d" % i, [128, 4, 2048], BF16) for i in range(2)]
        ch_w = [[P.chan() for _ in range(3)] for _ in range(2)]
        xe = [P.sb("xe%d" % i, [128, D], BF16) for i in range(2)]
        ch_xe = [P.chan(), P.chan()]
        XTj = [[P.sb("XT%d_%d" % (i, j), [128, 16, 128], BF16) for j in range(2)] for i in range(2)]
        ch_xt = [[P.chan(), P.chan()], [P.chan(), P.chan()]]
        sg = [P.sb("sg%d" % i, [128, 512], F32) for i in range(2)]
        actm = [P.sb("actm%d" % i, [128, 512], BF16) for i in range(2)]
        actT = [P.sb("actT%d" % i, [128, 4, 256], BF16) for i in range(2)]
        ye = [P.sb("ye%d" % i, [128, D], F32) for i in range(2)]
        ch_ye = [P.chan(), P.chan()]
        for e_ in range(NEXP):
            s = e_ % 2
            for q_ in range(4):
                P.dma("pool", DMA(wgt[s][:, q_ * 4:(q_ + 1) * 4, :], wgate_d[e_, :, :].rearrange("(k p) f -> p k f", p=128)[:, q_ * 4:(q_ + 1) * 4, :]),
                      ch_w[s][0], writes=[wgt[s].r] if q_ == 0 else [], accw=[] if q_ == 0 else [wgt[s].r])
                P.dma("pool", DMA(wut[s][:, q_ * 4:(q_ + 1) * 4, :], wup_d[e_, :, :].rearrange("(k p) f -> p k f", p=128)[:, q_ * 4:(q_ + 1) * 4, :]),
                      ch_w[s][1], writes=[wut[s].r] if q_ == 0 else [], accw=[] if q_ == 0 else [wut[s].r])
            P.dma("pool", DMA(wdt[s][:], wdown_d[e_, :, :].rearrange("(k p) f -> p k f", p=128)), ch_w[s][2], writes=[wdt[s].r])
            for j in range(2):
                P.dma("sp", (lambda e, j=j, e_=e_, s=s: e.dma_start_transpose(out=XTj[s][j][:, :, :],
                                                                           in_=hs[e_ * CAP + j * 128:e_ * CAP + (j + 1) * 128, :])),
                      ch_xt[s][j], reads=[R_hs], writes=[XTj[s][j].r])
            for j in range(2):
                bg = P.bank(); bu = P.bank()
                P.op("pe", [MM(bg[:, 0:512], XTj[s][j][:, k, :], wgt[s][:, k, :], k == 0, k == 15) for k in range(16)],
                     reads=[wgt[s].r, XTj[s][j].r], writes=[bg.r])
                P.op("pe", [MM(bu[:, 0:512], XTj[s][j][:, k, :], wut[s][:, k, :], k == 0, k == 15) for k in range(16)],
                     reads=[wut[s].r, XTj[s][j].r], writes=[bu.r])
                P.op("act", ACT(sg[j][:], bg[:, 0:512], AF.Silu), reads=[bg.r], writes=[sg[j].r])
                P.op("dve", TT(actm[j][:], sg[j][:], bu[:, 0:512], ALU.mult), reads=[sg[j].r, bu.r], writes=[actm[j].r])
                tp = P.tpbank()
                P.op("pe", [TR(tp[:, fc * 128:(fc + 1) * 128], actm[j][:, fc * 128:(fc + 1) * 128], ident) for fc in range(4)],
                     reads=[actm[j].r, cbf.r], writes=[tp.r])
                P.op("act", ACP(actT[s][:, :, j * 128:(j + 1) * 128], v3(tp[:, 0:512], 128)), reads=[tp.r], writes=[actT[s].r])
            for j in range(2):
                for db in range(4):
                    bank = P.bank()
                    P.op("pe", [MM(bank[:, 0:512], actT[s][:, fc, j * 128:(j + 1) * 128], wdt[s][:, fc, db * 512:(db + 1) * 512], fc == 0, fc == 3)
                                for fc in range(4)], reads=[actT[s].r, wdt[s].r], writes=[bank.r])
                    P.op("act", ACP(ye[j][:, db * 512:(db + 1) * 512], bank[:, 0:512]), reads=[bank.r], writes=[ye[j].r])
                P.dma("sp", DMA(ys[e_ * CAP + j * 128:e_ * CAP + (j + 1) * 128, :], ye[j][:]), ch_ye[j], reads=[ye[j].r], accw=[R_ys])
        P.barrier()
        if stop_after == 4:
            P.emit()
            ph.close()
            return nc, P
        ph.close()

        ph = ExitStack(); P.alloc = ph
        g1 = [P.sb("g1_%d" % i, [128, D], F32) for i in range(2)]
        g2 = [P.sb("g2_%d" % i, [128, D], F32) for i in range(2)]
        ot = [P.sb("ot%d" % i, [128, D], F32) for i in range(2)]
        ch_g = [[P.chan(), P.chan()], [P.chan(), P.chan()]]
        ch_o = [P.chan(), P.chan()]
        load_wn(2)
        for t in range(NT_HALF):
            s = t % 2
            P.op("dve", MSET(g1[s][:], 0.0), writes=[g1[s].r])
            P.op("dve", MSET(g2[s][:], 0.0), writes=[g2[s].r])
            P.dma("pool", GATHER(g1[s][:, :], ys[:, :], slots_i[:, 2 * t:2 * t + 1], NSLOT - 1), ch_g[s][0],
                  reads=[R_ys, slots_i.r], writes=[g1[s].r])
            P.dma("pool", GATHER(g2[s][:, :], ys[:, :], slots_i[:, 2 * t + 1:2 * t + 2], NSLOT - 1), ch_g[s][1],
                  reads=[R_ys, slots_i.r], writes=[g2[s].r])
            P.dma("sp", DMA(xt[s][:], x1buf[t * 128:(t + 1) * 128, :]), ch_x[s], reads=[R_x1], writes=[xt[s].r])
            P.op("dve", STT(xt[s][:], g1[s][:], cw[:, 2 * t:2 * t + 1], xt[s][:], ALU.mult, ALU.add),
                 reads=[g1[s].r, cw.r, xt[s].r], writes=[xt[s].r])
            P.op("dve", STT(xt[s][:], g2[s][:], cw[:, 2 * t + 1:2 * t + 2], xt[s][:], ALU.mult, ALU.add),
                 reads=[g2[s].r, cw.r, xt[s].r], writes=[xt[s].r])
            sN = ss[s]
            P.op("dve", MSET(sN[:, 0:1], 0.0), writes=[sN.r])
            P.op("act", ACT(junk[:], xt[s][:], AF.Square, scale=SC_D, accum_out=sN[:, 0:1]), reads=[xt[s].r], writes=[junk.r, sN.r])
            P.op("act", ACT(sN[:, 1:2], sN[:, 0:1], AF.Sqrt, bias=EPS), reads=[sN.r], writes=[sN.r])
            P.op("dve", RECIP(sN[:, 2:3], sN[:, 1:2]), reads=[sN.r], writes=[sN.r])
            P.op("dve", STT(ot[s][:], xt[s][:], sN[:, 2:3], wn[:], ALU.mult, ALU.mult), reads=[xt[s].r, sN.r, wn.r], writes=[ot[s].r])
            P.dma("sp", DMA(out_d[t * 128:(t + 1) * 128, :], ot[s][:]), ch_o[s], reads=[ot[s].r], accw=[R_out])
        P.emit()
        ph.close()
    return nc, P


def _consts():
    i = np.arange(128)
    J, I = np.meshgrid(i, i, indexing="ij")
    same = (J // 64) == (I // 64)
    cf = np.zeros((128, 1152), np.float32)
    cf[:, 0:128] = (J <= I)
    cf[:, 128:256] = (J > I)
    cf[:, 256:384] = np.where(I < J, -30000.0, 0.0)
    cf[:, 384:512] = np.eye(128)
    cf[:, 512:640] = np.where(same & (J <= I), -1.0 / 16.0, 0.0)
    cf[:, 640:768] = np.where(same & (J > I), -1.0 / 16.0, 0.0)
    m = np.where(same & (J <= I), 1.0, 0.0)
    cf[:, 768:896] = m
    cf[:, 896:1024] = m
    cf[:, 1024:1056] = (np.arange(32) * CAP)[None, :]
    cb = np.zeros((128, 384), np.float32)
    cb[:, 0:128] = np.eye(128)
    cb[:, 128:256] = (J < I)
    cb[:, 256:384] = 1.0
    sel = np.zeros((8, 8, 128), np.float32)
    for h in range(8):
        sel[h, h, :] = 1.0
    return cf, cb.astype(ml_dtypes.bfloat16), sel.reshape(8, 1024)


def _prep_inputs(inp):
    f = lambda k: np.asarray(inp[k], np.float32)
    x = f("x")
    w_in = f("w_in")[0]
    w_conv = f("w_conv")[0]; b_conv = f("b_conv")[0]
    w_gk_up = f("w_gk_up")[0]; b_gk = f("b_gk")[0]
    cf, cb, sel = _consts()
    wn3 = np.stack([np.broadcast_to(f("w_norm_mix")[0], (128, D)), np.broadcast_to(f("w_norm_ffn")[0], (128, D)),
                    np.broadcast_to(f("w_final_norm"), (128, D))]).astype(np.float32)
    wo = f("w_out")[0]
    perm = np.concatenate([np.arange(0, 512), np.arange(1024, 1536), np.arange(512, 1024), np.arange(1536, 2048)])
    wout = np.ascontiguousarray(wo[perm])
    wr = np.ascontiguousarray(np.concatenate([f("w_router_group")[0], f("w_router_expert")[0]], axis=1))
    br = np.concatenate([f("b_router_group")[0], f("b_router_expert")[0]])
    w_gate = f("w_gate")[0]; w_up = f("w_up")[0]; w_down = f("w_down")[0]
    maps = []
    for c in range(8):
        b, g = c // 2, c % 2
        qc = np.arange(256 * g, 256 * g + 256)
        kc = 512 + qc
        vc = 1024 + np.arange(512 * g, 512 * g + 512)
        gc = 2048 + np.arange(512 * g, 512 * g + 512)
        gkc = 3072 + np.arange(16)
        zc = 3088 + np.arange(512 * g, 512 * g + 512)
        xb0 = 4112
        xsl = np.arange(512 * g, 512 * g + 512)
        bml = 1024 + np.arange(256 * g, 256 * g + 256)
        cml = 1536 + np.arange(256 * g, 256 * g + 256)
        xbl = np.concatenate([xsl, bml, cml])
        dtc = 6160 + np.arange(8 * g, 8 * g + 8)
        wg_in = np.ascontiguousarray(w_in[:, np.concatenate([qc, kc, gkc, vc, gc, kc])])
        ws_in = np.ascontiguousarray(w_in[:, np.concatenate([xb0 + xbl, zc, dtc])])
        wgk = np.zeros((33, 256), np.float32)
        wgk[0:16] = w_gk_up[:, qc]
        wgk[32] = b_gk[qc]
        sm = np.zeros((128, 1024), np.float32)
        sm[:, 0:256] = f("w_gla_norm")[0][None, :]
        sm[:, 256:768] = f("w_ssd_norm")[0][512 * g:512 * g + 512][None, :]
        wcc = w_conv[:, xbl]
        sm[:, 768:800] = wcc.reshape(4, 8, 128).transpose(2, 1, 0).reshape(128, 32)
        sm[:, 800:808] = b_conv[xbl].reshape(8, 128).T
        sm[:, 808:816] = f("dt_bias")[0][8 * g:8 * g + 8][None, :]
        sm[:, 816:824] = f("a_log")[0][8 * g:8 * g + 8][None, :]
        sm[:, 824:832] = f("d_skip")[0][8 * g:8 * g + 8][None, :]
        sm[:, 832:868] = br[None, :]
        ridx = np.zeros((128, 32), np.int32)
        for t in range(16):
            for r in range(2):
                tok = 2048 * g + 128 * t + np.arange(128)
                ridx[:, 2 * t + r] = (tok // 512) * 1024 + r * 512 + tok % 512
        maps.append({
            "x_full": np.ascontiguousarray(x[b]), "x_half": np.ascontiguousarray(x[b, 2048 * g:2048 * g + 2048]),
            "wn3": wn3, "wg_in": wg_in, "ws_in": ws_in, "wout": wout, "wr": wr, "cf": cf, "cb": cb, "sel8": sel,
            "wgk": wgk, "smalls": sm, "ridx": ridx, "w_gate": w_gate, "w_up": w_up, "w_down": w_down,
        })
    return maps


def kernel(**inputs):
    maps = _prep_inputs(inputs)
    nc, _ = build_program(False)
    res = run_bass_kernel_spmd(nc, maps, core_ids=list(range(8)))
    out = np.zeros((4, SEQ, D), np.float32)
    for c in range(8):
        b, g = c // 2, c % 2
        out[b, 2048 * g:2048 * g + 2048] = np.asarray(res.results[c]["out"], np.float32)
    return out
```

```python
import numpy as np
import ml_dtypes
from contextlib import ExitStack
import concourse.bass as bass
import concourse.mybir as mybir
from concourse.bass_utils import run_bass_kernel_spmd

F32 = mybir.dt.float32
BF16 = mybir.dt.bfloat16
I32 = mybir.dt.int32
AF = mybir.ActivationFunctionType
ALU = mybir.AluOpType
AX = mybir.AxisListType

ENGS = ("pe", "act", "dve", "pool", "sp")
ENG_ATTR = {"pe": "tensor", "act": "scalar", "dve": "vector", "pool": "gpsimd", "sp": "sync"}

D = 2048
SEQ = 4096
NT_FULL = 32
NT_HALF = 16
CAP = 256
NEXP = 32
EPS = 1e-6
NSLOT = NEXP * CAP


class Res:
    __slots__ = ("name", "w", "r")

    def __init__(self, name):
        self.name = name
        self.w = {}
        self.r = {}


class Chan:
    def __init__(self, key):
        self.key = key
        self.n = 0


class Buf:
    def __init__(self, t, name):
        self.t = t
        self.r = Res(name)

    def __getitem__(self, k):
        return self.t[k]


class Prog:
    def __init__(self, nc):
        self.nc = nc
        self.ops = {e: [] for e in ENGS}
        self.cnt = {e: 0 for e in ENGS}
        self.waited = {e: {} for e in ENGS}
        self.chans = []
        self.stack = ExitStack()
        self.alloc = self.stack
        self.uid = 0
        self.banks = []
        self.tps = []
        self.bi = 0
        self.ti = 0
        self.ncc = 0

    def sb(self, name, shape, dtype):
        self.uid += 1
        nm = "sb%d_%s" % (self.uid, name)
        return Buf(self.alloc.enter_context(self.nc.sbuf_tensor(nm, list(shape), dtype)), nm)

    def ps(self, name, shape, dtype):
        return Buf(self.stack.enter_context(self.nc.psum_tensor("ps_" + name, list(shape), dtype)), name)

    def chan(self):
        c = Chan(len(self.chans))
        self.chans.append(c)
        return c

    def bank(self):
        b = self.banks[self.bi % len(self.banks)]
        self.bi += 1
        return b

    def tpbank(self):
        b = self.tps[self.ti % len(self.tps)]
        self.ti += 1
        return b

    def _deps(self, reads, writes, accw):
        d = {}

        def add(k, v):
            if d.get(k, 0) < v:
                d[k] = v
        for r in list(reads) + list(accw):
            for k, v in r.w.items():
                add(k, v)
        for w in writes:
            for k, v in w.w.items():
                add(k, v)
            for k, v in w.r.items():
                add(k, v)
        return d

    def _commit(self, tok, reads, writes, accw):
        k, v = tok
        for r in reads:
            if r.r.get(k, 0) < v:
                r.r[k] = v
        for w in writes:
            w.w = {k: v}
            w.r = {}
        for w in accw:
            if w.w.get(k, 0) < v:
                w.w[k] = v

    def _waitlist(self, eng, d, skip_self):
        wl = []
        for k, v in d.items():
            if skip_self and k == eng:
                continue
            if self.waited[eng].get(k, 0) >= v:
                continue
            self.waited[eng][k] = v
            wl.append((k, v))
        return wl

    def op(self, eng, fns, reads=(), writes=()):
        if callable(fns):
            fns = [fns]
        d = self._deps(reads, writes, ())
        wl = self._waitlist(eng, d, skip_self=(eng == "pe"))
        self.cnt[eng] += 1
        self._commit((eng, self.cnt[eng]), reads, writes, ())
        self.ops[eng].append((wl, fns, None))

    def dma(self, eng, fn, chan, reads=(), writes=(), accw=()):
        d = self._deps(reads, writes, accw)
        wl = self._waitlist(eng, d, skip_self=False)
        chan.n += 1
        self._commit((("c", chan.key), chan.n * 16), reads, writes, accw)
        self.ops[eng].append((wl, [fn], chan))

    def collective(self, fn, reads=(), writes=()):
        d = self._deps(reads, writes, ())
        wl = self._waitlist("pool", d, skip_self=False)
        key = ("cc", self.ncc)
        self.ncc += 1
        self._commit((key, 1), reads, writes, ())
        self.ops["pool"].append((wl, [fn], key))

    def barrier(self):
        for e in ENGS:
            d = {}
            for e2 in ENGS:
                if e2 != "sp" and self.cnt[e2]:
                    d[e2] = self.cnt[e2]
            for c in self.chans:
                if c.n:
                    d[("c", c.key)] = c.n * 16
            for i in range(self.ncc):
                d[("cc", i)] = 1
            wl = self._waitlist(e, d, skip_self=False)
            self.ops[e].append((wl, [], None))

    def emit(self):
        nc = self.nc
        with ExitStack() as st:
            sems = {}
            for e in ENGS:
                sems[e] = st.enter_context(nc.semaphore("s_" + e))
            for i in range(self.ncc):
                sems[("cc", i)] = st.enter_context(nc.semaphore("s_cc%d" % i))
            for c in self.chans:
                sems[("c", c.key)] = st.enter_context(nc.semaphore("c%d" % c.key))
            block = st.enter_context(nc.Block())
            for e in ENGS:
                def body(eng, e=e):
                    for wl, fns, chan in self.ops[e]:
                        for k, v in wl:
                            eng.wait_ge(sems[k], v)
                        if not fns:
                            continue
                        ins = None
                        for f in fns:
                            ins = f(eng)
                        if chan is None:
                            ins.then_inc(sems[e], 1)
                        elif isinstance(chan, tuple):
                            ins.then_inc(sems[chan])
                        else:
                            ins.then_inc(sems[("c", chan.key)], 16)
                    if e == "sp":
                        for c in self.chans:
                            if c.n:
                                eng.wait_ge(sems[("c", c.key)], c.n * 16)
                        for e2 in ENGS:
                            if e2 != "sp" and self.cnt[e2]:
                                eng.wait_ge(sems[e2], self.cnt[e2])
                getattr(block, ENG_ATTR[e])(body)


def MM(out, lhsT, rhs, start=True, stop=True):
    return lambda e: e.matmul(out, lhsT=lhsT, rhs=rhs, start=start, stop=stop)


def TR(out, in_, ident):
    return lambda e: e.transpose(out=out, in_=in_, identity=ident)


def ACT(out, in_, func, **kw):
    return lambda e: e.activation(out=out, in_=in_, func=func, **kw)


def ACP(out, in_):
    return lambda e: e.copy(out=out, in_=in_)


def AMUL(out, in_, m):
    return lambda e: e.mul(out=out, in_=in_, mul=m)


def TT(out, a, b, op):
    return lambda e: e.tensor_tensor(out=out, in0=a, in1=b, op=op)


def TS(out, a, s1, s2, op0, op1=None):
    if op1 is None:
        return lambda e: e.tensor_scalar(out=out, in0=a, scalar1=s1, scalar2=None, op0=op0)
    return lambda e: e.tensor_scalar(out=out, in0=a, scalar1=s1, scalar2=s2, op0=op0, op1=op1)


def STT(out, a, s, b, op0, op1):
    return lambda e: e.scalar_tensor_tensor(out=out, in0=a, scalar=s, in1=b, op0=op0, op1=op1)


def CP(out, in_):
    return lambda e: e.tensor_copy(out=out, in_=in_)


def MSET(ap, v):
    return lambda e: e.memset(ap, v)


def RECIP(out, in_):
    return lambda e: e.reciprocal(out=out, in_=in_)


def RMAX(out, in_):
    return lambda e: e.reduce_max(out=out, in_=in_, axis=AX.X)


def RSUM(out, in_):
    return lambda e: e.reduce_sum(out=out, in_=in_, axis=AX.X)


def DMA(out, in_):
    return lambda e: e.dma_start(out=out, in_=in_)


_BREG = {}


def _bound_reg(e, bound):
    key = (id(e), bound)
    if key not in _BREG:
        reg = e.alloc_register("bnd%d" % len(_BREG))
        e.reg_mov(reg, bound)
        _BREG[key] = reg
    return _BREG[key]


def GATHER(out, src, idx, bound):
    return lambda e: e.indirect_dma_start(out=out, out_offset=None, in_=src,
                                          in_offset=bass.IndirectOffsetOnAxis(ap=idx, axis=0),
                                          bounds_check=_bound_reg(e, bound), oob_is_err=False)


def SCATTER(dst, idx, src, bound):
    return lambda e: e.indirect_dma_start(out=dst, out_offset=bass.IndirectOffsetOnAxis(ap=idx, axis=0),
                                          in_=src, in_offset=None, bounds_check=_bound_reg(e, bound), oob_is_err=False)


def bc3(ap, m):
    n = ap.shape[1]
    return ap.unsqueeze(2).broadcast_to([128, n, m])


def v3(ap, d):
    return ap.rearrange("p (h d) -> p h d", d=d)


def build_program(dbg=False, stop_after=99):
    _BREG.clear()
    nc = bass.Bass("TRN2", target_bir_lowering=False)
    P = Prog(nc)

    def din(name, shape, dt=F32):
        return nc.dram_tensor(name, list(shape), dt, kind="ExternalInput")

    x_full = din("x_full", [SEQ, D])
    x_half = din("x_half", [2048, D])
    wn3_d = din("wn3", [3, 128, D])
    wg_in_d = din("wg_in", [D, 1808])
    ws_in_d = din("ws_in", [D, 1544])
    wout_d = din("wout", [D, D])
    wr_d = din("wr", [D, 36])
    cf_d = din("cf", [128, 1152])
    cb_d = din("cb", [128, 384], BF16)
    sel_d = din("sel8", [8, 1024])
    wgk_d = din("wgk", [33, 256])
    sm_d = din("smalls", [128, 1024])
    ridx_d = din("ridx", [128, 32], I32)
    ne_decl = NEXP if 3 <= stop_after <= 20 or stop_after == 99 else 1
    wgate_d = din("w_gate", [ne_decl, D, 512])
    wup_d = din("w_up", [ne_decl, D, 512])
    wdown_d = din("w_down", [ne_decl, 512, D])
    out_d = nc.dram_tensor("out", [2048, D], F32, kind="ExternalOutput")

    mixbuf = nc.dram_tensor("mixbuf", [SEQ, 1024], BF16)
    hTbuf = nc.dram_tensor("hTbuf", [8, 128, 16 * 512], BF16)
    R_hT = [Res("hTbuf%d" % i) for i in range(8)]
    gath = nc.dram_tensor("gath", [2 * SEQ, 1024], BF16)
    x1buf = nc.dram_tensor("x1buf", [2048, D], F32)
    hs = nc.dram_tensor("hs", [NSLOT, D], BF16)
    ys = nc.dram_tensor("ys", [NSLOT, D], F32)
    R_mix = [Res("mixbuf%d" % i) for i in range(8)]; R_gath = [Res("gath%d" % i) for i in range(8)]; R_x1 = Res("x1buf"); R_hs = Res("hs"); R_ys = Res("ys")
    R_out = Res("out")
    if dbg:
        dbg_mix = nc.dram_tensor("dbg_mix", [SEQ, 1024], BF16, kind="ExternalOutput")
        dbg_x1 = nc.dram_tensor("dbg_x1", [2048, D], F32, kind="ExternalOutput")
        dbg_slots = nc.dram_tensor("dbg_slots", [128, 32], I32, kind="ExternalOutput")
        dbg_cw = nc.dram_tensor("dbg_cw", [128, 32], F32, kind="ExternalOutput")

    with P.stack:
        P.tps = [P.ps("tp%d" % i, [128, 1024], BF16) for i in range(2)]
        P.banks = [P.ps("mm%d" % i, [128, 512], F32) for i in range(6)]

        cf = P.sb("cf", [128, 1152], F32)
        cbf = P.sb("cbf", [128, 384], BF16)
        wn = P.sb("wn", [128, D], F32)
        xt = [P.sb("xt%d" % i, [128, D], F32) for i in range(2)]
        hb = [P.sb("hb%d" % i, [128, D], BF16) for i in range(2)]
        junk = P.sb("junk", [128, D], BF16)
        ss = [P.sb("ss%d" % i, [128, 4], F32) for i in range(2)]
        smalls = P.sb("smalls", [128, 1024], F32)
        slots_i = P.sb("slots_i", [128, 32], I32)
        cw = P.sb("cw", [128, 32], F32)
        ch_x = [P.chan(), P.chan()]
        ch_c = P.chan()
        ch_wn = P.chan()
        P.dma("sp", DMA(cf[:], cf_d.ap()), P.chan(), writes=[cf.r])
        P.dma("sp", DMA(cbf[:], cb_d.ap()), P.chan(), writes=[cbf.r])
        P.dma("sp", DMA(smalls[:], sm_d.ap()), P.chan(), writes=[smalls.r])
        ident = cbf[:, 0:128]
        TriLt_b = cbf[:, 128:256]
        ones_b = cbf[:, 256:384]
        TriInc = cf[:, 0:128]; TriGt = cf[:, 128:256]; NegMask = cf[:, 256:384]; IdentF = cf[:, 384:512]
        TriIncBD = cf[:, 512:640]; TriGtBD = cf[:, 640:768]; MaskBD2 = cf[:, 768:1024]
        eC = cf[:, 1024:1056]
        wgn = smalls[:, 0:256]
        wsn = smalls[:, 256:768]
        wc = smalls[:, 768:800]
        bcv = smalls[:, 800:808]
        dtb = smalls[:, 808:816]; alog = smalls[:, 816:824]; dsk = smalls[:, 824:832]
        brb = smalls[:, 832:868]
        SC_D = float(1.0 / np.sqrt(D))

        def load_wn(i):
            P.dma("sp", DMA(wn[:], wn3_d[i, :, :]), ch_wn, writes=[wn.r])

        def rmsnorm_to_bf16(src, slot, dst):
            s = ss[slot]
            P.op("dve", MSET(s[:, 0:1], 0.0), writes=[s.r])
            P.op("act", ACT(junk[:], src[:], AF.Square, scale=SC_D, accum_out=s[:, 0:1]),
                 reads=[src.r], writes=[junk.r, s.r])
            P.op("act", ACT(s[:, 1:2], s[:, 0:1], AF.Sqrt, bias=EPS), reads=[s.r], writes=[s.r])
            P.op("dve", RECIP(s[:, 2:3], s[:, 1:2]), reads=[s.r], writes=[s.r])
            P.op("dve", STT(dst[:], src[:], s[:, 2:3], wn[:], ALU.mult, ALU.mult),
                 reads=[src.r, s.r, wn.r], writes=[dst.r])

        def transpose16(src, dstT, col0):
            for half in range(2):
                tp = P.tpbank()
                P.op("pe", [TR(tp[:, c * 128:(c + 1) * 128], src[:, (half * 8 + c) * 128:(half * 8 + c + 1) * 128], ident)
                            for c in range(8)], reads=[src.r, cbf.r], writes=[tp.r])
                eng = "act" if half == 0 else "dve"
                f = ACP if eng == "act" else CP
                P.op(eng, f(dstT[:, half * 8:(half + 1) * 8, col0:col0 + 128], v3(tp[:, :], 128)),
                     reads=[tp.r], writes=[dstT.r])

        def front_block(blk, hT):
            for tt in range(4):
                t = blk * 4 + tt
                s = t % 2
                P.dma("sp", DMA(xt[s][:], x_full[t * 128:(t + 1) * 128, :]), ch_x[s], writes=[xt[s].r])
                rmsnorm_to_bf16(xt[s], s, hb[s])
                transpose16(hb[s], hT, tt * 128)

        def load_w(dram, ncols, name):
            W = P.sb(name, [128, 16, ncols], BF16)
            parts = []
            src = dram.ap().rearrange("(k p) n -> p k n", p=128)
            for q in range(4):
                r = Res("%s_%d" % (name, q))
                P.dma("pool", DMA(W[:, q * 4:(q + 1) * 4, :], src[:, q * 4:(q + 1) * 4, :]), P.chan(), writes=[r])
                parts.append(r)
            return W, parts

        def fm_group(W, Wr, c0, M, hT, bank):
            P.op("pe", [MM(bank[0:M, 0:512], W[:, k, c0:c0 + M], hT[:, k, :], k == 0, k == 15) for k in range(16)],
                 reads=Wr + [hT.r], writes=[bank.r])

        def tm_group(W, Wr, c0, N, hT, tt, bank):
            P.op("pe", [MM(bank[:, 0:N], hT[:, k, tt * 128:(tt + 1) * 128], W[:, k, c0:c0 + N], k == 0, k == 15)
                        for k in range(16)], reads=Wr + [hT.r], writes=[bank.r])

        load_wn(0)
        if stop_after == 0:
            P.emit()
            return nc, P

        ph = ExitStack(); P.alloc = ph
        Ws, Wsr = load_w(ws_in_d, 1544, "Ws")
        hTs = [P.sb("hT%d" % i, [128, 16, 512], BF16) for i in range(2)]
        xraw = [P.sb("xraw%d" % c, [128, 515], F32) for c in range(8)]
        acc = [P.sb("acc%d" % i, [128, 512], F32) for i in range(2)]
        xcT = [[P.sb("xcT%d_%d" % (i, c), [128, 512], BF16) for c in range(8)] for i in range(2)]
        zs = [P.sb("zs%d" % i, [128, 512], BF16) for i in range(8)]
        dtv = [P.sb("dtv%d" % i, [128, 32], F32) for i in range(8)]
        ed = [P.sb("ed%d" % i, [128, 16], F32) for i in range(2)]
        na = [P.sb("na%d" % i, [128, 8], F32) for i in range(2)]
        acs = [P.sb("acs%d" % i, [128, 8], F32) for i in range(2)]
        acsT = [P.sb("acsT%d" % i, [8, 128], F32) for i in range(2)]
        sel8 = P.sb("sel8", [8, 1024], F32)
        ea = P.sb("ea", [128, 8], F32)
        eAl = [P.sb("eAl%d" % i, [128, 8], F32) for i in range(2)]
        Lh = [P.sb("Lh%d" % i, [128, 128], F32) for i in range(3)]
        Lcl = [P.sb("Lcl%d" % i, [128, 128], F32) for i in range(3)]
        MT = [P.sb("MT%d" % i, [128, 128], BF16) for i in range(4)]
        cbs = [P.sb("cbs%d" % i, [128, 256], F32) for i in range(2)]
        xtm = [P.sb("xtm%d" % i, [128, 768], BF16) for i in range(2)]
        xsdt = [P.sb("xsdt%d" % i, [128, 512], BF16) for i in range(2)]
        xsdec = [P.sb("xsdec%d" % i, [128, 512], BF16) for i in range(2)]
        ysb = [P.sb("ysb%d" % i, [128, 512], F32) for i in range(2)]
        ytmp = P.sb("ytmp", [128, 512], F32)
        RT = P.sb("RT", [128, 512], F32)
        RTb = P.sb("RTb", [128, 512], BF16)
        nss = [P.sb("nss%d" % i, [128, 8], F32) for i in range(2)]
        ob = [P.sb("ob%d" % i, [128, 512], BF16) for i in range(2)]
        ch_ob = [P.chan(), P.chan()]

        P.dma("sp", DMA(sel8[:], sel_d.ap()), P.chan(), writes=[sel8.r])
        P.op("act", ACT(ea[:], alog, AF.Exp), reads=[smalls.r], writes=[ea.r])
        P.op("dve", MSET(RT[:], 0.0), writes=[RT.r])
        P.op("dve", MSET(RTb[:], 0.0), writes=[RTb.r])
        for c in range(8):
            P.op("dve", MSET(xraw[c][:, 0:3], 0.0), writes=[xraw[c].r])

        def front_tile(blk, tt, hT):
            t = blk * 4 + tt
            s = t % 2
            P.dma("sp", DMA(xt[s][:], x_full[t * 128:(t + 1) * 128, :]), ch_x[s], writes=[xt[s].r])
            rmsnorm_to_bf16(xt[s], s, hb[s])
            transpose16(hb[s], hT, tt * 128)

        def ssd_fm(blk, c):
            hT = hTs[blk % 2]
            xc = xcT[blk % 2][c]
            bank = P.bank()
            fm_group(Ws, Wsr, c * 128, 128, hT, bank)
            P.op("act", ACP(xraw[c][:, 3:515], bank[:, 0:512]), reads=[bank.r], writes=[xraw[c].r])
            a = acc[c % 2]
            P.op("dve", TS(a[:], xraw[c][:, 3:515], wc[:, c * 4 + 3:c * 4 + 4], bcv[:, c:c + 1], ALU.mult, ALU.add),
                 reads=[xraw[c].r, smalls.r], writes=[a.r])
            for k in (2, 1, 0):
                P.op("dve", STT(a[:], xraw[c][:, k:k + 512], wc[:, c * 4 + k:c * 4 + k + 1], a[:], ALU.mult, ALU.add),
                     reads=[xraw[c].r, smalls.r, a.r], writes=[a.r])
            P.op("act", ACT(xc[:], a[:], AF.Silu), reads=[a.r], writes=[xc.r])
            P.op("dve", CP(xraw[c][:, 0:3], xraw[c][:, 512:515]), reads=[xraw[c].r], writes=[xraw[c].r])

        def ssd_tm(blk, tt):
            hT = hTs[blk % 2]
            z_ = zs[(blk % 2) * 4 + tt]
            d_ = dtv[(blk % 2) * 4 + tt]
            bank = P.bank()
            tm_group(Ws, Wsr, 1024, 512, hT, tt, bank)
            P.op("act", ACT(z_[:], bank[:, 0:512], AF.Silu), reads=[bank.r], writes=[z_.r])
            bank = P.bank()
            tm_group(Ws, Wsr, 1536, 8, hT, tt, bank)
            P.op("dve", TT(d_[:, 0:8], bank[:, 0:8], dtb, ALU.add), reads=[bank.r, smalls.r], writes=[d_.r])
            P.op("act", ACT(d_[:, 8:16], d_[:, 0:8], AF.Exp), reads=[d_.r], writes=[d_.r])
            P.op("act", ACT(d_[:, 16:24], d_[:, 8:16], AF.Ln, bias=1.0), reads=[d_.r], writes=[d_.r])
            P.op("dve", STT(d_[:, 24:32], d_[:, 16:24], -1.0, ea[:], ALU.mult, ALU.mult),
                 reads=[d_.r, ea.r], writes=[d_.r])

        def ssd_s2(blk, tt):
            t = blk * 4 + tt
            s = t % 2
            tc_ = slice(tt * 128, (tt + 1) * 128)
            xcT_ = xcT[blk % 2]
            z_ = zs[(blk % 2) * 4 + tt]
            d_ = dtv[(blk % 2) * 4 + tt]
            dt_ = d_[:, 16:24]; dA = d_[:, 24:32]
            bank = P.bank()
            P.op("pe", [MM(bank[:, 0:8], TriInc, dA), MM(bank[:, 8:16], TriGt, dA)],
                 reads=[cf.r, d_.r], writes=[bank.r])
            P.op("act", ACT(ed[s][:], bank[:, 0:16], AF.Exp), reads=[bank.r], writes=[ed[s].r])
            P.op("dve", TS(na[s][:], bank[:, 0:8], -1.0, None, ALU.mult), reads=[bank.r], writes=[na[s].r])
            P.op("act", ACP(acs[s][:], bank[:, 0:8]), reads=[bank.r], writes=[acs[s].r])
            bank = P.bank()
            P.op("pe", MM(bank[0:8, 0:128], acs[s][:], IdentF), reads=[acs[s].r, cf.r], writes=[bank.r])
            P.op("act", ACP(acsT[s][:], bank[0:8, 0:128]), reads=[bank.r], writes=[acsT[s].r])
            bank = P.bank()
            P.op("pe", [MM(bank[:, gi * 128:(gi + 1) * 128], xcT_[4 + gi][:, tc_], xcT_[6 + gi][:, tc_]) for gi in range(2)],
                 reads=[xcT_[4].r, xcT_[5].r, xcT_[6].r, xcT_[7].r], writes=[bank.r])
            P.op("dve", TT(v3(cbs[s][:, :], 128), v3(bank[:, 0:256], 128), TriInc.unsqueeze(1).broadcast_to([128, 2, 128]), ALU.mult),
                 reads=[bank.r, cf.r], writes=[cbs[s].r])
            tp = P.tpbank()
            P.op("pe", [TR(tp[:, c * 128:(c + 1) * 128], xcT_[c][:, tc_], ident) for c in range(6)],
                 reads=[xcT_[c].r for c in range(6)] + [cbf.r], writes=[tp.r])
            P.op("act", ACP(xtm[s][:], tp[:, 0:768]), reads=[tp.r], writes=[xtm[s].r])
            P.op("dve", TT(v3(xsdt[s][:, :], 64), v3(xtm[s][:, 0:512], 64), bc3(dt_, 64), ALU.mult),
                 reads=[xtm[s].r, d_.r], writes=[xsdt[s].r])
            P.op("dve", TT(v3(xsdec[s][:, :], 64), v3(xsdt[s][:, :], 64), bc3(ed[s][:, 8:16], 64), ALU.mult),
                 reads=[xsdt[s].r, ed[s].r], writes=[xsdec[s].r])
            bankY = P.bank(); bankO = P.bank(); bankS = P.bank()
            P.op("pe", [MM(bankO[:, gi * 256:(gi + 1) * 256], xcT_[6 + gi][:, tc_], RTb[:, gi * 256:(gi + 1) * 256]) for gi in range(2)]
                 + [MM(bankS[:, gi * 256:(gi + 1) * 256], xtm[s][:, 512 + gi * 128:512 + (gi + 1) * 128], xsdec[s][:, gi * 256:(gi + 1) * 256])
                    for gi in range(2)],
                 reads=[xcT_[6].r, xcT_[7].r, RTb.r, xtm[s].r, xsdec[s].r], writes=[bankO.r, bankS.r])
            for hq in range(2):
                bankA = P.bank()
                fns = []
                for h4 in range(4):
                    h = hq * 4 + h4
                    fns.append(MM(bankA[:, h4 * 128:(h4 + 1) * 128], sel8[:, h * 128:(h + 1) * 128], acsT[s][:], True, True))
                P.op("pe", fns, reads=[sel8.r, acsT[s].r, cf.r], writes=[bankA.r])
                P.op("act", ACT(eAl[s][:, hq * 4:(hq + 1) * 4], v3(bankA[:, :], 128)[:, :, 127], AF.Exp),
                     reads=[bankA.r], writes=[eAl[s].r])
                for h4 in range(4):
                    h = hq * 4 + h4
                    gi = h // 4
                    L = Lh[h % 3]; M_ = MT[h % 4]
                    Lc = Lcl[h % 3]
                    P.op("dve", TS(Lc[:], bankA[:, h4 * 128:(h4 + 1) * 128], na[s][:, h:h + 1], 0.0, ALU.add, ALU.min),
                         reads=[bankA.r, na[s].r], writes=[Lc.r])
                    P.op("act", ACT(L[:], Lc[:], AF.Exp), reads=[Lc.r], writes=[L.r])
                    P.op("dve", TT(M_[:], L[:], cbs[s][:, gi * 128:(gi + 1) * 128], ALU.mult),
                         reads=[L.r, cbs[s].r], writes=[M_.r])
                    hc = slice(h * 64, (h + 1) * 64)
                    P.op("pe", MM(bankY[:, hc], M_[:], xsdt[s][:, hc]), reads=[M_.r, xsdt[s].r], writes=[bankY.r])
            y_ = ysb[s]
            P.op("dve", TT(v3(y_[:, :], 64), v3(bankO[:, 0:512], 64), bc3(ed[s][:, 0:8], 64), ALU.mult),
                 reads=[bankO.r, ed[s].r], writes=[y_.r])
            P.op("dve", TT(y_[:], y_[:], bankY[:, 0:512], ALU.add), reads=[y_.r, bankY.r], writes=[y_.r])
            P.op("dve", TT(v3(ytmp[:, :], 64), v3(xtm[s][:, 0:512], 64), bc3(dsk, 64), ALU.mult),
                 reads=[xtm[s].r, smalls.r], writes=[ytmp.r])
            P.op("dve", TT(y_[:], y_[:], ytmp[:], ALU.add), reads=[y_.r, ytmp.r], writes=[y_.r])
            P.op("dve", TT(y_[:], y_[:], z_[:], ALU.mult), reads=[y_.r, z_.r], writes=[y_.r])
            P.op("dve", TT(v3(RT[:, :], 64), v3(RT[:, :], 64), bc3(eAl[s][:, :], 64), ALU.mult),
                 reads=[RT.r, eAl[s].r], writes=[RT.r])
            P.op("dve", TT(RT[:], RT[:], bankS[:, 0:512], ALU.add), reads=[RT.r, bankS.r], writes=[RT.r])
            P.op("act", ACP(RTb[:], RT[:]), reads=[RT.r], writes=[RTb.r])
            n_ = nss[s]
            P.op("dve", MSET(n_[:, 0:2], 0.0), writes=[n_.r])
            for gi in range(2):
                P.op("act", ACT(junk[:, 0:256], y_[:, gi * 256:(gi + 1) * 256], AF.Square, scale=1.0 / 16.0,
                                accum_out=n_[:, gi:gi + 1]), reads=[y_.r], writes=[junk.r, n_.r])
            P.op("act", ACT(n_[:, 2:4], n_[:, 0:2], AF.Sqrt, bias=EPS), reads=[n_.r], writes=[n_.r])
            P.op("dve", RECIP(n_[:, 4:6], n_[:, 2:4]), reads=[n_.r], writes=[n_.r])
            for gi in range(2):
                P.op("dve", STT(ob[s][:, gi * 256:(gi + 1) * 256], y_[:, gi * 256:(gi + 1) * 256], n_[:, 4 + gi:5 + gi],
                                wsn[:, gi * 256:(gi + 1) * 256], ALU.mult, ALU.mult),
                     reads=[y_.r, n_.r, smalls.r], writes=[ob[s].r])
            P.dma("sp", DMA(mixbuf[t * 128:(t + 1) * 128, 512:1024], ob[s][:]), ch_ob[s], reads=[ob[s].r], accw=[R_mix[blk]])

        ch_hts = [P.chan(), P.chan()]

        def spill_hT(blk):
            hT = hTs[blk % 2]
            P.dma("sp", DMA(hTbuf[blk, :, :].rearrange("p (k t) -> p k t", k=16), hT[:, :, :]), ch_hts[blk % 2],
                  reads=[hT.r], accw=[R_hT[blk]])

        for tt in range(4):
            front_tile(0, tt, hTs[0])
        spill_hT(0)
        for c in range(8):
            ssd_fm(0, c)
        for tt in range(4):
            ssd_tm(0, tt)
        for tt in range(4):
            front_tile(1, tt, hTs[1])
        spill_hT(1)
        for blk in range(8):
            for tt in range(4):
                if blk + 1 < 8:
                    ssd_fm(blk + 1, 2 * tt)
                    ssd_fm(blk + 1, 2 * tt + 1)
                    ssd_tm(blk + 1, tt)
                if blk + 2 < 8:
                    front_tile(blk + 2, tt, hTs[blk % 2])
                    if tt == 3:
                        spill_hT(blk + 2)
                ssd_s2(blk, tt)
        P.barrier()
        if stop_after == 1:
            if dbg:
                chd = P.chan()
                for i_ in range(32):
                    P.dma("sp", DMA(dbg_mix[i_ * 128:(i_ + 1) * 128, :], mixbuf[i_ * 128:(i_ + 1) * 128, :]), chd, reads=R_mix)
            P.emit()
            ph.close()
            return nc, P
        ph.close()

        ph = ExitStack(); P.alloc = ph
        Wg, Wgr = load_w(wg_in_d, 1808, "Wg")
        hTs = [P.sb("hT%d" % i, [128, 16, 512], BF16) for i in range(2)]
        qT = [P.sb("qT%d" % i, [128, 2, 512], F32) for i in range(2)]
        kT = [P.sb("kT%d" % i, [128, 2, 512], F32) for i in range(2)]
        gkT = [P.sb("gkT%d" % i, [33, 512], BF16) for i in range(2)]
        wgk_f = P.sb("wgk_f", [33, 256], F32)
        wgk = P.sb("wgk", [33, 256], BF16)
        vb = [P.sb("vb%d" % i, [128, 512], BF16) for i in range(8)]
        gs = [P.sb("gs%d" % i, [128, 512], BF16) for i in range(8)]
        ktm = [P.sb("ktm%d" % i, [128, 256], BF16) for i in range(8)]
        sp1 = [P.sb("sp1%d" % i, [128, 256], F32) for i in range(2)]
        spl = [P.sb("spl%d" % i, [128, 256], F32) for i in range(2)]
        eG = [P.sb("eG%d" % i, [128, 256], F32) for i in range(2)]
        eGn = [P.sb("eGn%d" % i, [128, 256], F32) for i in range(2)]
        eD2 = [P.sb("eD2%d" % i, [128, 256], F32) for i in range(2)]
        qtf = [P.sb("qtf%d" % i, [128, 256], BF16) for i in range(2)]
        ktf = [P.sb("ktf%d" % i, [128, 256], BF16) for i in range(2)]
        khat = [P.sb("khat%d" % i, [128, 256], BF16) for i in range(2)]
        qlo = [P.sb("qlo%d" % i, [128, 256], BF16) for i in range(2)]
        qhi = [P.sb("qhi%d" % i, [128, 256], BF16) for i in range(2)]
        Sm = [P.sb("Sm%d" % i, [128, 256], BF16) for i in range(2)]
        Sf = P.sb("Sf", [128, 512], F32); Sb = P.sb("Sb", [128, 512], BF16)
        Sfm = P.sb("Sfm", [128, 512], F32); Sbm = P.sb("Sbm", [128, 512], BF16)
        otmp = [P.sb("otmp%d" % i, [128, 512], F32) for i in range(2)]
        nss = [P.sb("nss%d" % i, [128, 8], F32) for i in range(2)]
        ob = [P.sb("ob%d" % i, [128, 512], BF16) for i in range(2)]
        ch_ob = [P.chan(), P.chan()]

        P.dma("sp", DMA(wgk_f[:], wgk_d.ap()), P.chan(), writes=[wgk_f.r])
        P.op("dve", CP(wgk[:], wgk_f[:]), reads=[wgk_f.r], writes=[wgk.r])
        for i in range(2):
            P.op("dve", MSET(gkT[i][0:32, :], 0.0), writes=[gkT[i].r])
            P.op("dve", MSET(gkT[i][32:33, :], 1.0), writes=[gkT[i].r])
        P.op("dve", MSET(Sf[:], 0.0), writes=[Sf.r])
        P.op("dve", MSET(Sb[:], 0.0), writes=[Sb.r])
        for i in range(2):
            P.op("dve", MSET(qlo[i][:], 0.0), writes=[qlo[i].r])
            P.op("dve", MSET(qhi[i][:], 0.0), writes=[qhi[i].r])

        def gla_fm(blk, which):
            hT = hTs[blk % 2]
            qT_ = qT[blk % 2]; kT_ = kT[blk % 2]; gkT_ = gkT[blk % 2]
            bank = P.bank()
            if which < 2:
                hh = which
                fm_group(Wg, Wgr, hh * 128, 128, hT, bank)
                P.op("act", AMUL(qT_[:, hh, :], bank[:, 0:512], float(128.0 ** -0.5)), reads=[bank.r], writes=[qT_.r])
            elif which < 4:
                hh = which - 2
                fm_group(Wg, Wgr, 256 + hh * 128, 128, hT, bank)
                P.op("act", ACP(kT_[:, hh, :], bank[:, 0:512]), reads=[bank.r], writes=[kT_.r])
            else:
                fm_group(Wg, Wgr, 512, 16, hT, bank)
                P.op("act", ACP(gkT_[0:16, :], bank[0:16, 0:512]), reads=[bank.r], writes=[gkT_.r])

        def gla_tm(blk, tt):
            hT = hTs[blk % 2]
            i_ = (blk % 2) * 4 + tt
            bank = P.bank()
            tm_group(Wg, Wgr, 528, 512, hT, tt, bank)
            P.op("act", ACP(vb[i_][:], bank[:, 0:512]), reads=[bank.r], writes=[vb[i_].r])
            bank = P.bank()
            tm_group(Wg, Wgr, 1040, 512, hT, tt, bank)
            P.op("act", ACT(gs[i_][:], bank[:, 0:512], AF.Silu), reads=[bank.r], writes=[gs[i_].r])
            bank = P.bank()
            tm_group(Wg, Wgr, 1552, 256, hT, tt, bank)
            P.op("act", ACP(ktm[i_][:], bank[:, 0:256]), reads=[bank.r], writes=[ktm[i_].r])

        def gla_s2(blk, tt):
            t = blk * 4 + tt
            s = t % 2
            tc_ = slice(tt * 128, (tt + 1) * 128)
            qT_ = qT[blk % 2]; kT_ = kT[blk % 2]; gkT_ = gkT[blk % 2]
            i_ = (blk % 2) * 4 + tt
            vb_ = vb[i_]; gs_ = gs[i_]; ktm_ = ktm[i_]
            bank = P.bank()
            P.op("pe", MM(bank[:, 0:256], gkT_[0:33, tc_], wgk[:]), reads=[gkT_.r, wgk.r], writes=[bank.r])
            P.op("act", ACT(sp1[s][:], bank[:, 0:256], AF.Exp, scale=-1.0), reads=[bank.r], writes=[sp1[s].r])
            P.op("act", ACT(spl[s][:], sp1[s][:], AF.Ln, bias=1.0), reads=[sp1[s].r], writes=[spl[s].r])
            bank = P.bank()
            P.op("pe", [MM(bank[:, hh * 128:(hh + 1) * 128], spl[s][:, hh * 128:(hh + 1) * 128], TriIncBD) for hh in range(2)],
                 reads=[spl[s].r, cf.r], writes=[bank.r])
            P.op("act", ACT(eG[s][:], bank[:, 0:256], AF.Exp), reads=[bank.r], writes=[eG[s].r])
            P.op("act", ACT(eGn[s][:], bank[:, 0:256], AF.Exp, scale=-1.0), reads=[bank.r], writes=[eGn[s].r])
            bank = P.bank()
            P.op("pe", MM(bank[:, 0:256], TriGtBD, spl[s][:]), reads=[spl[s].r, cf.r], writes=[bank.r])
            P.op("act", ACT(eD2[s][:], bank[:, 0:256], AF.Exp), reads=[bank.r], writes=[eD2[s].r])
            P.op("dve", TT(v3(qtf[s][:, :], 128), qT_[:, :, tc_], v3(eG[s][:, :], 128), ALU.mult),
                 reads=[qT_.r, eG[s].r], writes=[qtf[s].r])
            P.op("dve", TT(v3(ktf[s][:, :], 128), kT_[:, :, tc_], v3(eGn[s][:, :], 128), ALU.mult),
                 reads=[kT_.r, eGn[s].r], writes=[ktf[s].r])
            P.op("dve", TT(khat[s][:], ktm_[:], eD2[s][:], ALU.mult), reads=[ktm_.r, eD2[s].r], writes=[khat[s].r])
            P.op("act", ACP(v3(qlo[s][:, :], 128)[:, :, 0:64], v3(qtf[s][:, :], 128)[:, :, 0:64]),
                 reads=[qtf[s].r], writes=[qlo[s].r])
            P.op("act", ACP(v3(qhi[s][:, :], 128)[:, :, 64:128], v3(qtf[s][:, :], 128)[:, :, 64:128]),
                 reads=[qtf[s].r], writes=[qhi[s].r])
            bank = P.bank()
            P.op("pe", [MM(bank[:, hh * 128:(hh + 1) * 128], ktf[s][:, hh * 128:(hh + 1) * 128], qtf[s][:, hh * 128:(hh + 1) * 128])
                        for hh in range(2)], reads=[ktf[s].r, qtf[s].r], writes=[bank.r])
            P.op("dve", TT(Sm[s][:], bank[:, 0:256], MaskBD2, ALU.mult), reads=[bank.r, cf.r], writes=[Sm[s].r])
            bankT = P.bank()
            P.op("pe", [MM(bankT[:, hh * 256:(hh + 1) * 256], khat[s][0:64, hh * 128:(hh + 1) * 128], vb_[0:64, hh * 256:(hh + 1) * 256])
                        for hh in range(2)], reads=[khat[s].r, vb_.r], writes=[bankT.r])
            for hh in range(2):
                P.op("dve", STT(Sfm[:, hh * 256:(hh + 1) * 256], Sf[:, hh * 256:(hh + 1) * 256],
                                eG[s][:, hh * 128 + 63:hh * 128 + 64], bankT[:, hh * 256:(hh + 1) * 256], ALU.mult, ALU.add),
                     reads=[Sf.r, eG[s].r, bankT.r], writes=[Sfm.r])
            P.op("act", ACP(Sbm[:], Sfm[:]), reads=[Sfm.r], writes=[Sbm.r])
            bankO = P.bank()
            fns = []
            for hh in range(2):
                oc = slice(hh * 256, (hh + 1) * 256); hc = slice(hh * 128, (hh + 1) * 128)
                fns.append(MM(bankO[:, oc], Sm[s][:, hc], vb_[:, oc], True, False))
                fns.append(MM(bankO[:, oc], qlo[s][:, hc], Sb[:, oc], False, False))
                fns.append(MM(bankO[:, oc], qhi[s][:, hc], Sbm[:, oc], False, True))
            P.op("pe", fns, reads=[Sm[s].r, vb_.r, qlo[s].r, qhi[s].r, Sb.r, Sbm.r], writes=[bankO.r])
            bankT = P.bank()
            P.op("pe", [MM(bankT[:, hh * 256:(hh + 1) * 256], khat[s][64:128, hh * 128:(hh + 1) * 128], vb_[64:128, hh * 256:(hh + 1) * 256])
                        for hh in range(2)], reads=[khat[s].r, vb_.r], writes=[bankT.r])
            for hh in range(2):
                P.op("dve", STT(Sf[:, hh * 256:(hh + 1) * 256], Sfm[:, hh * 256:(hh + 1) * 256],
                                eG[s][:, hh * 128 + 127:hh * 128 + 128], bankT[:, hh * 256:(hh + 1) * 256], ALU.mult, ALU.add),
                     reads=[Sfm.r, eG[s].r, bankT.r], writes=[Sf.r])
            P.op("act", ACP(Sb[:], Sf[:]), reads=[Sf.r], writes=[Sb.r])
            n_ = nss[s]
            P.op("dve", MSET(n_[:, 0:2], 0.0), writes=[n_.r])
            for hh in range(2):
                P.op("act", ACT(junk[:, 0:256], bankO[:, hh * 256:(hh + 1) * 256], AF.Square, scale=1.0 / 16.0,
                                accum_out=n_[:, hh:hh + 1]), reads=[bankO.r], writes=[junk.r, n_.r])
            P.op("act", ACT(n_[:, 2:4], n_[:, 0:2], AF.Sqrt, bias=EPS), reads=[n_.r], writes=[n_.r])
            P.op("dve", RECIP(n_[:, 4:6], n_[:, 2:4]), reads=[n_.r], writes=[n_.r])
            for hh in range(2):
                P.op("dve", STT(otmp[s][:, hh * 256:(hh + 1) * 256], bankO[:, hh * 256:(hh + 1) * 256], n_[:, 4 + hh:5 + hh],
                                wgn, ALU.mult, ALU.mult), reads=[bankO.r, n_.r, smalls.r], writes=[otmp[s].r])
            P.op("dve", TT(ob[s][:], otmp[s][:], gs_[:], ALU.mult), reads=[otmp[s].r, gs_.r], writes=[ob[s].r])
            P.dma("sp", DMA(mixbuf[t * 128:(t + 1) * 128, 0:512], ob[s][:]), ch_ob[s], reads=[ob[s].r], accw=[R_mix[blk]])

        ch_htl = [P.chan(), P.chan()]

        def reload_hT(blk):
            hT = hTs[blk % 2]
            P.dma("sp", DMA(hT[:, :, :], hTbuf[blk, :, :].rearrange("p (k t) -> p k t", k=16)), ch_htl[blk % 2],
                  reads=[R_hT[blk]], writes=[hT.r])

        reload_hT(0)
        for w_ in range(5):
            gla_fm(0, w_)
        for tt in range(4):
            gla_tm(0, tt)
        reload_hT(1)
        for blk in range(8):
            for tt in range(4):
                if blk + 1 < 8:
                    for w_ in ((0, 1), (2, 3), (4,), ())[tt]:
                        gla_fm(blk + 1, w_)
                    gla_tm(blk + 1, tt)
                if blk + 2 < 8 and tt == 0:
                    reload_hT(blk + 2)
                gla_s2(blk, tt)
            P.collective(lambda e, blk=blk: e.collective_compute("AllGather", ALU.bypass,
                                                                 replica_groups=[[0, 1], [2, 3], [4, 5], [6, 7]],
                                                                 ins=[mixbuf[blk * 512:(blk + 1) * 512, :].opt()],
                                                                 outs=[gath[blk * 1024:(blk + 1) * 1024, :].opt()]),
                         reads=[R_mix[blk]], writes=[R_gath[blk]])
        P.barrier()
        if stop_after == 2:
            if dbg:
                chd = P.chan()
                for i_ in range(32):
                    P.dma("sp", DMA(dbg_mix[i_ * 128:(i_ + 1) * 128, :], mixbuf[i_ * 128:(i_ + 1) * 128, :]), chd, reads=R_mix)
            P.emit()
            ph.close()
            return nc, P
        ph.close()

        if dbg:
            chd = P.chan()
            for i_ in range(32):
                P.dma("sp", DMA(dbg_mix[i_ * 128:(i_ + 1) * 128, :], mixbuf[i_ * 128:(i_ + 1) * 128, :]), chd, reads=R_mix)
        if stop_after == 21:
            P.barrier()
            P.emit()
            return nc, P
        ph = ExitStack(); P.alloc = ph
        Wo, Wor = load_w(wout_d, 2048, "Wo")
        Wr, Wrr = load_w(wr_d, 36, "Wr")
        ridx = P.sb("ridx", [128, 32], I32)
        mt = [P.sb("mt%d" % i, [128, 2048], BF16) for i in range(2)]
        mixT = [P.sb("mixT%d" % i, [128, 16, 128], BF16) for i in range(2)]
        x1 = [P.sb("x1_%d" % i, [128, D], F32) for i in range(2)]
        h2T = [P.sb("h2T%d" % i, [128, 16, 128], BF16) for i in range(2)]
        rt = [P.sb("rt%d" % i, [128, 256], F32) for i in range(2)]
        Ab = [P.sb("Ab%d" % i, [128, 32], BF16) for i in range(2)]
        cntb = P.sb("cntb", [128, 32], F32)
        slots_f = P.sb("slots_f", [128, 32], F32)
        ch_mt = [[P.chan(), P.chan()], [P.chan(), P.chan()]]
        ch_x1 = [P.chan(), P.chan()]
        ch_sc = [[P.chan(), P.chan()], [P.chan(), P.chan()]]
        P.dma("sp", DMA(ridx[:], ridx_d.ap()), P.chan(), writes=[ridx.r])
        P.op("dve", MSET(cntb[:], 0.0), writes=[cntb.r])
        load_wn(1)
        def b_s1a(t):
            s = t % 2
            rmt = [Res("mt%d_%d" % (s, r)) for r in range(2)]
            for r in range(2):
                P.dma("pool", GATHER(mt[s][:, r * 1024:(r + 1) * 1024], gath[:, :], ridx[:, 2 * t + r:2 * t + r + 1], 2 * SEQ - 1),
                      ch_mt[s][r], reads=[R_gath[t // 4], R_gath[4 + t // 4], ridx.r], writes=[mt[s].r] if r == 0 else [], accw=[] if r == 0 else [mt[s].r])
            P.dma("sp", DMA(xt[s][:], x_half[t * 128:(t + 1) * 128, :]), ch_x[s], writes=[xt[s].r])
            transpose16(mt[s], mixT[s], 0)
            obanks = []
            for db in range(4):
                bank = P.bank()
                obanks.append(bank)
                P.op("pe", [MM(bank[:, 0:512], mixT[s][:, k, :], Wo[:, k, db * 512:(db + 1) * 512], k == 0, k == 15) for k in range(16)],
                     reads=Wor + [mixT[s].r], writes=[bank.r])
            return obanks

        def b_s1b(t, obanks):
            s = t % 2
            for db in range(4):
                bank = obanks[db]
                P.op("dve", TT(x1[s][:, db * 512:(db + 1) * 512], bank[:, 0:512], xt[s][:, db * 512:(db + 1) * 512], ALU.add),
                     reads=[bank.r, xt[s].r], writes=[x1[s].r])
            P.dma("sp", DMA(x1buf[t * 128:(t + 1) * 128, :], x1[s][:]), ch_x1[s], reads=[x1[s].r], accw=[R_x1])
            if dbg:
                P.dma("sp", DMA(dbg_x1[t * 128:(t + 1) * 128, :], x1[s][:]), ch_x1[s], reads=[x1[s].r])
            rmsnorm_to_bf16(x1[s], s, hb[s])
            transpose16(hb[s], h2T[s], 0)
            bank = P.bank()
            P.op("pe", [MM(bank[:, 0:36], h2T[s][:, k, :], Wr[:, k, 0:36], k == 0, k == 15) for k in range(16)],
                 reads=Wrr + [h2T[s].r], writes=[bank.r])
            q = rt[s]; qr = [q.r]
            L = q[:, 0:36]
            P.op("dve", TT(L, bank[:, 0:36], brb, ALU.add), reads=[bank.r, smalls.r], writes=qr)

        def b_s2(t):
            s = t % 2
            q = rt[s]; qr = [q.r]
            L = q[:, 0:36]
            gmax = q[:, 36:37]; ngmax = q[:, 37:38]; gsum = q[:, 38:39]; gw = q[:, 39:40]
            gexp = q[:, 40:44]; ohg = q[:, 44:48]; esel = q[:, 48:56]; m1 = q[:, 56:57]; oh1 = q[:, 57:65]
            esel2 = q[:, 65:73]; m2 = q[:, 73:74]; oh2 = q[:, 74:82]; dd = q[:, 82:83]; e2 = q[:, 83:84]
            rden = q[:, 84:85]; OH1 = q[:, 88:120]; OH2 = q[:, 120:152]; pos = q[:, 152:184]; tmpa = q[:, 184:216]
            psel = q[:, 216:218]; ssum = q[:, 218:220]; ovf = q[:, 220:222]; Af = q[:, 222:254]
            P.op("dve", RMAX(gmax, L[:, 0:4]), reads=qr, writes=qr)
            P.op("dve", TS(ngmax, gmax, -1.0, None, ALU.mult), reads=qr, writes=qr)
            P.op("dve", MSET(gsum, 0.0), reads=qr, writes=qr)
            P.op("act", ACT(gexp, L[:, 0:4], AF.Exp, bias=ngmax, accum_out=gsum), reads=qr, writes=qr)
            P.op("dve", RECIP(gw, gsum), reads=qr, writes=qr)
            P.op("dve", TS(ohg, L[:, 0:4], gmax, None, ALU.is_equal), reads=qr, writes=qr)
            P.op("dve", TS(esel, L[:, 4:12], ohg[:, 0:1], None, ALU.mult), reads=qr, writes=qr)
            for g_ in range(1, 4):
                P.op("dve", STT(esel, L[:, 4 + 8 * g_:12 + 8 * g_], ohg[:, g_:g_ + 1], esel, ALU.mult, ALU.add), reads=qr, writes=qr)
            P.op("dve", RMAX(m1, esel), reads=qr, writes=qr)
            P.op("dve", TS(oh1, esel, m1, None, ALU.is_equal), reads=qr, writes=qr)
            P.op("dve", STT(esel2, oh1, -1e30, esel, ALU.mult, ALU.add), reads=qr, writes=qr)
            P.op("dve", RMAX(m2, esel2), reads=qr, writes=qr)
            P.op("dve", TS(oh2, esel2, m2, None, ALU.is_equal), reads=qr, writes=qr)
            P.op("dve", TT(dd, m2, m1, ALU.subtract), reads=qr, writes=qr)
            P.op("act", ACT(e2, dd, AF.Exp), reads=qr, writes=qr)
            P.op("dve", TS(rden, e2, 1.0, None, ALU.add), reads=qr, writes=qr)
            P.op("dve", RECIP(rden, rden), reads=qr, writes=qr)
            P.op("dve", TT(cw[:, 2 * t:2 * t + 1], gw, rden, ALU.mult), reads=qr, writes=[cw.r])
            P.op("dve", TT(cw[:, 2 * t + 1:2 * t + 2], cw[:, 2 * t:2 * t + 1], e2, ALU.mult), reads=qr + [cw.r], writes=[cw.r])
            for g_ in range(4):
                P.op("dve", TS(OH1[:, 8 * g_:8 * g_ + 8], oh1, ohg[:, g_:g_ + 1], None, ALU.mult), reads=qr, writes=qr)
                P.op("dve", TS(OH2[:, 8 * g_:8 * g_ + 8], oh2, ohg[:, g_:g_ + 1], None, ALU.mult), reads=qr, writes=qr)
            P.op("dve", TT(Af, OH1, OH2, ALU.add), reads=qr, writes=qr)
            P.op("dve", CP(Ab[s][:], Af), reads=qr, writes=[Ab[s].r])
            bank = P.bank()
            P.op("pe", [MM(bank[:, 0:32], TriLt_b, Ab[s][:]), MM(bank[:, 32:64], ones_b, Ab[s][:])],
                 reads=[cbf.r, Ab[s].r], writes=[bank.r])
            P.op("dve", TT(pos, bank[:, 0:32], cntb[:], ALU.add), reads=[bank.r, cntb.r], writes=qr)
            P.op("dve", TT(cntb[:], cntb[:], bank[:, 32:64], ALU.add), reads=[bank.r, cntb.r], writes=[cntb.r])
            for j, OH in enumerate((OH1, OH2)):
                P.op("dve", TT(tmpa, pos, OH, ALU.mult), reads=qr, writes=qr)
                P.op("dve", RSUM(psel[:, j:j + 1], tmpa), reads=qr, writes=qr)
                P.op("dve", TT(tmpa, eC, OH, ALU.mult), reads=qr + [cf.r], writes=qr)
                P.op("dve", RSUM(ssum[:, j:j + 1], tmpa), reads=qr, writes=qr)
            P.op("dve", TS(ovf, psel, float(CAP) - 0.5, 1.0e6, ALU.is_ge, ALU.mult), reads=qr, writes=qr)
            P.op("dve", TT(ssum, ssum, psel, ALU.add), reads=qr, writes=qr)
            P.op("dve", TT(slots_f[:, 2 * t:2 * t + 2], ssum, ovf, ALU.add), reads=qr, writes=[slots_f.r])
            P.op("dve", CP(slots_i[:, 2 * t:2 * t + 2], slots_f[:, 2 * t:2 * t + 2]), reads=[slots_f.r], writes=[slots_i.r])
            for j in range(2):
                P.dma("pool", SCATTER(hs[:, :], slots_i[:, 2 * t + j:2 * t + j + 1], hb[s][:, :], NSLOT - 1),
                      ch_sc[s][j], reads=[hb[s].r, slots_i.r], accw=[R_hs])

        ob_ = b_s1a(0)
        b_s1b(0, ob_)
        for t in range(NT_HALF):
            if t + 1 < NT_HALF:
                ob_ = b_s1a(t + 1)
            b_s2(t)
            if t + 1 < NT_HALF:
                b_s1b(t + 1, ob_)
        if dbg:
            P.dma("sp", DMA(dbg_slots.ap(), slots_i[:]), P.chan(), reads=[slots_i.r])
            P.dma("sp", DMA(dbg_cw.ap(), cw[:]), P.chan(), reads=[cw.r])
        P.barrier()
        if stop_after == 3:
            P.emit()
            ph.close()
            return nc, P
        ph.close()

        ph = ExitStack(); P.alloc = ph
        wgt = [P.sb("wgt%d" % i, [128, 16, 512], BF16) for i in range(2)]
        wut = [P.sb("wut%d" % i, [128, 16, 512], BF16) for i in range(2)]
        wdt = [P.sb("wdt%d" % i, [128, 4, 2048], BF16) for i in range(2)]
        ch_w = [[P.chan() for _ in range(3)] for _ in range(2)]
        xe = [P.sb("xe%d" % i, [128, D], BF16) for i in range(2)]
        ch_xe = [P.chan(), P.chan()]
        XT = [P.sb("XT%d" % i, [128, 16, 256], BF16) for i in range(2)]
        sg = [P.sb("sg%d" % i, [128, 512], F32) for i in range(2)]
        actm = [P.sb("actm%d" % i, [128, 512], BF16) for i in range(2)]
        actT = [P.sb("actT%d" % i, [128, 4, 256], BF16) for i in range(2)]
        ye = [P.sb("ye%d" % i, [128, D], F32) for i in range(2)]
        ch_ye = [P.chan(), P.chan()]
        for e_ in range(NEXP):
            s = e_ % 2
            for q_ in range(4):
                P.dma("pool", DMA(wgt[s][:, q_ * 4:(q_ + 1) * 4, :], wgate_d[e_, :, :].rearrange("(k p) f -> p k f", p=128)[:, q_ * 4:(q_ + 1) * 4, :]),
                      ch_w[s][0], writes=[wgt[s].r] if q_ == 0 else [], accw=[] if q_ == 0 else [wgt[s].r])
                P.dma("pool", DMA(wut[s][:, q_ * 4:(q_ + 1) * 4, :], wup_d[e_, :, :].rearrange("(k p) f -> p k f", p=128)[:, q_ * 4:(q_ + 1) * 4, :]),
                      ch_w[s][1], writes=[wut[s].r] if q_ == 0 else [], accw=[] if q_ == 0 else [wut[s].r])
            P.dma("pool", DMA(wdt[s][:], wdown_d[e_, :, :].rearrange("(k p) f -> p k f", p=128)), ch_w[s][2], writes=[wdt[s].r])
            for j in range(2):
                P.dma("sp", DMA(xe[j][:], hs[e_ * CAP + j * 128:e_ * CAP + (j + 1) * 128, :]), ch_xe[j], reads=[R_hs], writes=[xe[j].r])
                transpose16(xe[j], XT[s], j * 128)
            for j in range(2):
                bg = P.bank(); bu = P.bank()
                P.op("pe", [MM(bg[:, 0:512], XT[s][:, k, j * 128:(j + 1) * 128], wgt[s][:, k, :], k == 0, k == 15) for k in range(16)],
                     reads=[wgt[s].r, XT[s].r], writes=[bg.r])
                P.op("pe", [MM(bu[:, 0:512], XT[s][:, k, j * 128:(j + 1) * 128], wut[s][:, k, :], k == 0, k == 15) for k in range(16)],
                     reads=[wut[s].r, XT[s].r], writes=[bu.r])
                P.op("act", ACT(sg[j][:], bg[:, 0:512], AF.Silu), reads=[bg.r], writes=[sg[j].r])
                P.op("dve", TT(actm[j][:], sg[j][:], bu[:, 0:512], ALU.mult), reads=[sg[j].r, bu.r], writes=[actm[j].r])
                tp = P.tpbank()
                P.op("pe", [TR(tp[:, fc * 128:(fc + 1) * 128], actm[j][:, fc * 128:(fc + 1) * 128], ident) for fc in range(4)],
                     reads=[actm[j].r, cbf.r], writes=[tp.r])
                P.op("act", ACP(actT[s][:, :, j * 128:(j + 1) * 128], v3(tp[:, 0:512], 128)), reads=[tp.r], writes=[actT[s].r])
            for j in range(2):
                for db in range(4):
                    bank = P.bank()
                    P.op("pe", [MM(bank[:, 0:512], actT[s][:, fc, j * 128:(j + 1) * 128], wdt[s][:, fc, db * 512:(db + 1) * 512], fc == 0, fc == 3)
                                for fc in range(4)], reads=[actT[s].r, wdt[s].r], writes=[bank.r])
                    P.op("act", ACP(ye[j][:, db * 512:(db + 1) * 512], bank[:, 0:512]), reads=[bank.r], writes=[ye[j].r])
                P.dma("sp", DMA(ys[e_ * CAP + j * 128:e_ * CAP + (j + 1) * 128, :], ye[j][:]), ch_ye[j], reads=[ye[j].r], accw=[R_ys])
        P.barrier()
        if stop_after == 4:
            P.emit()
            ph.close()
            return nc, P
        ph.close()

        ph = ExitStack(); P.alloc = ph
        g1 = [P.sb("g1_%d" % i, [128, D], F32) for i in range(2)]
        g2 = [P.sb("g2_%d" % i, [128, D], F32) for i in range(2)]
        ot = [P.sb("ot%d" % i, [128, D], F32) for i in range(2)]
        ch_g = [[P.chan(), P.chan()], [P.chan(), P.chan()]]
        ch_o = [P.chan(), P.chan()]
        load_wn(2)
        for t in range(NT_HALF):
            s = t % 2
            P.op("dve", MSET(g1[s][:], 0.0), writes=[g1[s].r])
            P.op("dve", MSET(g2[s][:], 0.0), writes=[g2[s].r])
            P.dma("pool", GATHER(g1[s][:, :], ys[:, :], slots_i[:, 2 * t:2 * t + 1], NSLOT - 1), ch_g[s][0],
                  reads=[R_ys, slots_i.r], writes=[g1[s].r])
            P.dma("pool", GATHER(g2[s][:, :], ys[:, :], slots_i[:, 2 * t + 1:2 * t + 2], NSLOT - 1), ch_g[s][1],
                  reads=[R_ys, slots_i.r], writes=[g2[s].r])
            P.dma("sp", DMA(xt[s][:], x1buf[t * 128:(t + 1) * 128, :]), ch_x[s], reads=[R_x1], writes=[xt[s].r])
            P.op("dve", STT(xt[s][:], g1[s][:], cw[:, 2 * t:2 * t + 1], xt[s][:], ALU.mult, ALU.add),
                 reads=[g1[s].r, cw.r, xt[s].r], writes=[xt[s].r])
            P.op("dve", STT(xt[s][:], g2[s][:], cw[:, 2 * t + 1:2 * t + 2], xt[s][:], ALU.mult, ALU.add),
                 reads=[g2[s].r, cw.r, xt[s].r], writes=[xt[s].r])
            sN = ss[s]
            P.op("dve", MSET(sN[:, 0:1], 0.0), writes=[sN.r])
            P.op("act", ACT(junk[:], xt[s][:], AF.Square, scale=SC_D, accum_out=sN[:, 0:1]), reads=[xt[s].r], writes=[junk.r, sN.r])
            P.op("act", ACT(sN[:, 1:2], sN[:, 0:1], AF.Sqrt, bias=EPS), reads=[sN.r], writes=[sN.r])
            P.op("dve", RECIP(sN[:, 2:3], sN[:, 1:2]), reads=[sN.r], writes=[sN.r])
            P.op("dve", STT(ot[s][:], xt[s][:], sN[:, 2:3], wn[:], ALU.mult, ALU.mult), reads=[xt[s].r, sN.r, wn.r], writes=[ot[s].r])
            P.dma("sp", DMA(out_d[t * 128:(t + 1) * 128, :], ot[s][:]), ch_o[s], reads=[ot[s].r], accw=[R_out])
        P.emit()
        ph.close()
    return nc, P


def _consts():
    i = np.arange(128)
    J, I = np.meshgrid(i, i, indexing="ij")
    same = (J // 64) == (I // 64)
    cf = np.zeros((128, 1152), np.float32)
    cf[:, 0:128] = (J <= I)
    cf[:, 128:256] = (J > I)
    cf[:, 256:384] = np.where(I < J, -30000.0, 0.0)
    cf[:, 384:512] = np.eye(128)
    cf[:, 512:640] = np.where(same & (J <= I), -1.0 / 16.0, 0.0)
    cf[:, 640:768] = np.where(same & (J > I), -1.0 / 16.0, 0.0)
    m = np.where(same & (J <= I), 1.0, 0.0)
    cf[:, 768:896] = m
    cf[:, 896:1024] = m
    cf[:, 1024:1056] = (np.arange(32) * CAP)[None, :]
    cb = np.zeros((128, 384), np.float32)
    cb[:, 0:128] = np.eye(128)
    cb[:, 128:256] = (J < I)
    cb[:, 256:384] = 1.0
    sel = np.zeros((8, 8, 128), np.float32)
    for h in range(8):
        sel[h, h, :] = 1.0
    return cf, cb.astype(ml_dtypes.bfloat16), sel.reshape(8, 1024)


def _prep_inputs(inp):
    f = lambda k: np.asarray(inp[k], np.float32)
    x = f("x")
    w_in = f("w_in")[0]
    w_conv = f("w_conv")[0]; b_conv = f("b_conv")[0]
    w_gk_up = f("w_gk_up")[0]; b_gk = f("b_gk")[0]
    cf, cb, sel = _consts()
    wn3 = np.stack([np.broadcast_to(f("w_norm_mix")[0], (128, D)), np.broadcast_to(f("w_norm_ffn")[0], (128, D)),
                    np.broadcast_to(f("w_final_norm"), (128, D))]).astype(np.float32)
    wo = f("w_out")[0]
    perm = np.concatenate([np.arange(0, 512), np.arange(1024, 1536), np.arange(512, 1024), np.arange(1536, 2048)])
    wout = np.ascontiguousarray(wo[perm])
    wr = np.ascontiguousarray(np.concatenate([f("w_router_group")[0], f("w_router_expert")[0]], axis=1))
    br = np.concatenate([f("b_router_group")[0], f("b_router_expert")[0]])
    w_gate = f("w_gate")[0]; w_up = f("w_up")[0]; w_down = f("w_down")[0]
    maps = []
    for c in range(8):
        b, g = c // 2, c % 2
        qc = np.arange(256 * g, 256 * g + 256)
        kc = 512 + qc
        vc = 1024 + np.arange(512 * g, 512 * g + 512)
        gc = 2048 + np.arange(512 * g, 512 * g + 512)
        gkc = 3072 + np.arange(16)
        zc = 3088 + np.arange(512 * g, 512 * g + 512)
        xb0 = 4112
        xsl = np.arange(512 * g, 512 * g + 512)
        bml = 1024 + np.arange(256 * g, 256 * g + 256)
        cml = 1536 + np.arange(256 * g, 256 * g + 256)
        xbl = np.concatenate([xsl, bml, cml])
        dtc = 6160 + np.arange(8 * g, 8 * g + 8)
        wg_in = np.ascontiguousarray(w_in[:, np.concatenate([qc, kc, gkc, vc, gc, kc])])
        ws_in = np.ascontiguousarray(w_in[:, np.concatenate([xb0 + xbl, zc, dtc])])
        wgk = np.zeros((33, 256), np.float32)
        wgk[0:16] = w_gk_up[:, qc]
        wgk[32] = b_gk[qc]
        sm = np.zeros((128, 1024), np.float32)
        sm[:, 0:256] = f("w_gla_norm")[0][None, :]
        sm[:, 256:768] = f("w_ssd_norm")[0][512 * g:512 * g + 512][None, :]
        wcc = w_conv[:, xbl]
        sm[:, 768:800] = wcc.reshape(4, 8, 128).transpose(2, 1, 0).reshape(128, 32)
        sm[:, 800:808] = b_conv[xbl].reshape(8, 128).T
        sm[:, 808:816] = f("dt_bias")[0][8 * g:8 * g + 8][None, :]
        sm[:, 816:824] = f("a_log")[0][8 * g:8 * g + 8][None, :]
        sm[:, 824:832] = f("d_skip")[0][8 * g:8 * g + 8][None, :]
        sm[:, 832:868] = br[None, :]
        ridx = np.zeros((128, 32), np.int32)
        for t in range(16):
            for r in range(2):
                tok = 2048 * g + 128 * t + np.arange(128)
                ridx[:, 2 * t + r] = (tok // 512) * 1024 + r * 512 + tok % 512
        maps.append({
            "x_full": np.ascontiguousarray(x[b]), "x_half": np.ascontiguousarray(x[b, 2048 * g:2048 * g + 2048]),
            "wn3": wn3, "wg_in": wg_in, "ws_in": ws_in, "wout": wout, "wr": wr, "cf": cf, "cb": cb, "sel8": sel,
            "wgk": wgk, "smalls": sm, "ridx": ridx, "w_gate": w_gate, "w_up": w_up, "w_down": w_down,
        })
    return maps


def kernel(**inputs):
    maps = _prep_inputs(inputs)
    nc, _ = build_program(False)
    res = run_bass_kernel_spmd(nc, maps, core_ids=list(range(8)))
    out = np.zeros((4, SEQ, D), np.float32)
    for c in range(8):
        b, g = c // 2, c % 2
        out[b, 2048 * g:2048 * g + 2048] = np.asarray(res.results[c]["out"], np.float32)
    return out
```
